# Optimizing a Trainium2 kernel written in Bass

```python
import math
import jax, jax.numpy as jnp
from jax import lax
import numpy as np

D_MODEL = 1024
BATCH = 8
SEQ = 2048
DEPTH = 4

ATTN_HEADS = 8
HEAD_DIM = 64
ATTN_WIDTH = ATTN_HEADS * HEAD_DIM
CONV_GROUPS = 8
CONV_WIDTH = D_MODEL - ATTN_WIDTH
IN_COLS = 3 * ATTN_WIDTH + 2 * CONV_WIDTH
MOBA_BLOCK = 256
MOBA_TOPK = 3
Q_CHUNK = 64
ROPE_THETA = 500000.0
ROT_DIM = HEAD_DIM // 4
CONV_KERNEL = 31
N_EXPERTS = 32
TOP_K = 4
D_FF = D_MODEL
SWIGLU_LIMIT = 7.0
SWIGLU_ALPHA = 1.702
MOE_BLOCK = 256
DEEPNORM_ALPHA = (2.0 * DEPTH) ** 0.25
DEEPNORM_BETA = (8.0 * DEPTH) ** -0.25
LN_EPS = 1e-5

kernel_name = "hybrid_moba_conformer_moe_deepnorm"


def layer_norm(x, g, b):
    xf = x.astype(jnp.float32)
    mu = jnp.mean(xf, axis=-1, keepdims=True)
    var = jnp.mean(jnp.square(xf - mu), axis=-1, keepdims=True)
    return ((xf - mu) * lax.rsqrt(var + LN_EPS) * g + b).astype(x.dtype)


def rms_norm(x, g):
    xf = x.astype(jnp.float32)
    return (xf * lax.rsqrt(jnp.mean(jnp.square(xf), axis=-1, keepdims=True) + LN_EPS) * g).astype(x.dtype)


def rotary_tables(positions):
    inv_freq = ROPE_THETA ** (-jnp.arange(0, ROT_DIM, 2, dtype=jnp.float32) / ROT_DIM)
    ang = positions.astype(jnp.float32)[..., None] * inv_freq
    return jnp.cos(ang)[:, :, None, :], jnp.sin(ang)[:, :, None, :]


def partial_rotary(x, cos, sin):
    xr = x[..., :ROT_DIM].astype(jnp.float32)
    x1, x2 = xr[..., :ROT_DIM // 2], xr[..., ROT_DIM // 2:]
    rot = jnp.concatenate([x1 * cos - x2 * sin, x2 * cos + x1 * sin], axis=-1)
    return jnp.concatenate([rot.astype(x.dtype), x[..., ROT_DIM:]], axis=-1)


def moba_attention(q, k, v):
    B, H, S, Dh = q.shape
    nb = -(-S // MOBA_BLOCK)
    pad = nb * MOBA_BLOCK - S
    kp = jnp.pad(k, ((0, 0), (0, 0), (0, pad), (0, 0)))
    vp = jnp.pad(v, ((0, 0), (0, 0), (0, pad), (0, 0)))
    k_blocks = kp.reshape(B, H, nb, MOBA_BLOCK, Dh)
    v_blocks = vp.reshape(B, H, nb, MOBA_BLOCK, Dh)
    n_valid = jnp.clip(S - jnp.arange(nb) * MOBA_BLOCK, 1, MOBA_BLOCK).astype(jnp.float32)
    k_mean = (jnp.sum(k_blocks.astype(jnp.float32), axis=3) / n_valid[:, None]).astype(k.dtype)
    topk = min(MOBA_TOPK, nb)
    scale = HEAD_DIM ** -0.5
    n_chunks = S // Q_CHUNK
    q_chunks = q.reshape(B, H, n_chunks, Q_CHUNK, Dh).transpose(2, 0, 1, 3, 4)
    bi = jnp.arange(B)[:, None, None, None]
    hi = jnp.arange(H)[None, :, None, None]
    blk_ids = jnp.arange(nb)

    def chunk_fn(args):
        c, qc = args
        q_start = c * Q_CHUNK
        own = q_start // MOBA_BLOCK
        gate = jnp.einsum('bhqd,bhnd->bhqn', qc, k_mean)
        gate = jnp.where(blk_ids < own, gate, -jnp.inf)
        _, gidx = lax.top_k(gate, topk)
        gvalid = gidx < own
        k_sel = k_blocks[bi, hi, gidx]
        v_sel = v_blocks[bi, hi, gidx]
        s_sel = jnp.einsum('bhqd,bhqnkd->bhqnk', qc, k_sel).astype(jnp.float32) * scale
        s_sel = jnp.where(gvalid[..., None], s_sel, -jnp.inf).reshape(B, H, Q_CHUNK, topk * MOBA_BLOCK)
        k_own = lax.dynamic_index_in_dim(k_blocks, own, axis=2, keepdims=False)
        v_own = lax.dynamic_index_in_dim(v_blocks, own, axis=2, keepdims=False)
        s_own = jnp.einsum('bhqd,bhkd->bhqk', qc, k_own).astype(jnp.float32) * scale
        q_pos = q_start + jnp.arange(Q_CHUNK)
        k_pos = own * MOBA_BLOCK + jnp.arange(MOBA_BLOCK)
        s_own = jnp.where(k_pos[None, :] <= q_pos[:, None], s_own, -jnp.inf)
        p = jax.nn.softmax(jnp.concatenate([s_sel, s_own], axis=-1), axis=-1).astype(v.dtype)
        p_sel = p[..., :topk * MOBA_BLOCK].reshape(B, H, Q_CHUNK, topk, MOBA_BLOCK)
        p_own = p[..., topk * MOBA_BLOCK:]
        return (jnp.einsum('bhqnk,bhqnkd->bhqd', p_sel, v_sel)
                + jnp.einsum('bhqk,bhkd->bhqd', p_own, v_own))

    out = lax.map(chunk_fn, (jnp.arange(n_chunks), q_chunks))
    return out.transpose(1, 0, 3, 2, 4).reshape(B, S, H * Dh)


def conformer_conv(u2, conv_w, conv_b, ln_g, ln_b):
    a, g = jnp.split(u2, 2, axis=-1)
    u = a * jax.nn.sigmoid(g)
    u = lax.conv_general_dilated(
        u, conv_w[:, None, :].astype(u.dtype), window_strides=(1,),
        padding=[(CONV_KERNEL - 1, 0)], dimension_numbers=('NWC', 'WIO', 'NWC'),
        feature_group_count=CONV_WIDTH) + conv_b
    return jax.nn.silu(layer_norm(u, ln_g, ln_b))


def moe_ffn(x2d, w_router, b_router, w_gu, b_gu, w_dn, b_dn):
    T, D = x2d.shape
    TK = T * TOP_K
    logits = (x2d @ w_router + b_router).astype(jnp.float32)
    top_val, top_idx = lax.top_k(logits, TOP_K)
    gates = jax.nn.softmax(top_val, axis=-1).astype(x2d.dtype)
    flat_e = top_idx.reshape(-1)
    flat_tok = jnp.arange(TK, dtype=jnp.int32) // TOP_K
    order = jnp.argsort(flat_e)
    sorted_e = flat_e[order]
    sorted_tok = flat_tok[order]
    counts = jnp.bincount(flat_e, length=N_EXPERTS)
    padded = ((counts + MOE_BLOCK - 1) // MOE_BLOCK) * MOE_BLOCK
    pad_end = jnp.cumsum(padded)
    pad_start = pad_end - padded
    grp_start = jnp.cumsum(counts) - counts
    dest = pad_start[sorted_e] + (jnp.arange(TK) - grp_start[sorted_e])
    n_blocks = TK // MOE_BLOCK + N_EXPERTS
    buf_tok = jnp.full((n_blocks * MOE_BLOCK,), T, jnp.int32).at[dest].set(sorted_tok)
    block_e = jnp.clip(jnp.searchsorted(pad_end, jnp.arange(n_blocks) * MOE_BLOCK, side='right'),
                       0, N_EXPERTS - 1)
    x_pad = jnp.concatenate([x2d, jnp.zeros((1, D), x2d.dtype)], axis=0)

    def block_fn(args):
        tok, e = args
        xb = x_pad[tok]
        h = xb @ w_gu[e] + b_gu[e]
        x_glu, x_lin = jnp.split(h, 2, axis=-1)
        x_glu = jnp.minimum(x_glu, SWIGLU_LIMIT)
        x_lin = jnp.clip(x_lin, -SWIGLU_LIMIT, SWIGLU_LIMIT)
        act = x_glu * jax.nn.sigmoid(SWIGLU_ALPHA * x_glu) * (x_lin + 1.0)
        return act @ w_dn[e] + b_dn[e]

    y_buf = lax.map(block_fn, (buf_tok.reshape(n_blocks, MOE_BLOCK), block_e))
    y_assign = y_buf.reshape(-1, D)[dest]
    g_sorted = gates.reshape(-1)[order]
    return jax.ops.segment_sum(y_assign * g_sorted[:, None], sorted_tok, num_segments=T)


def setup_inputs(seed: int = 0) -> dict:
    key = jax.random.key(seed)
    ks = jax.random.split(key, 20)
    L, D = DEPTH, D_MODEL
    f32 = jnp.float32

    def nrm(k, shape, scale):
        return jax.random.normal(k, shape, f32) * scale

    x = jax.random.normal(ks[0], (BATCH, SEQ, D), f32)
    positions = jnp.broadcast_to(jnp.arange(SEQ, dtype=jnp.int32)[None, :], (BATCH, SEQ))
    w_in = nrm(ks[1], (L, D, IN_COLS), D ** -0.5)
    w_in = w_in.at[:, :, 2 * ATTN_WIDTH:3 * ATTN_WIDTH].multiply(DEEPNORM_BETA)
    attn_gain = 1.0 + nrm(ks[2], (L, ATTN_WIDTH), 0.01)
    conv_w = nrm(ks[3], (L, CONV_KERNEL, CONV_WIDTH), CONV_KERNEL ** -0.5)
    conv_b = nrm(ks[4], (L, CONV_WIDTH), 0.01)
    conv_ln_g = 1.0 + nrm(ks[5], (L, CONV_WIDTH), 0.01)
    conv_ln_b = nrm(ks[6], (L, CONV_WIDTH), 0.01)
    w_o = nrm(ks[7], (L, D, D), D ** -0.5 * DEEPNORM_BETA)
    ln1_g = 1.0 + nrm(ks[8], (L, D), 0.01)
    ln1_b = nrm(ks[9], (L, D), 0.01)
    w_router = nrm(ks[10], (L, D, N_EXPERTS), D ** -0.5)
    b_router = nrm(ks[11], (L, N_EXPERTS), 0.01)
    w_gu = nrm(ks[12], (L, N_EXPERTS, D, 2 * D_FF), D ** -0.5)
    b_gu = nrm(ks[13], (L, N_EXPERTS, 2 * D_FF), 0.01)
    w_dn = nrm(ks[14], (L, N_EXPERTS, D_FF, D), D_FF ** -0.5 * DEEPNORM_BETA)
    b_dn = nrm(ks[15], (L, N_EXPERTS, D), 0.01)
    ln2_g = 1.0 + nrm(ks[16], (L, D), 0.01)
    ln2_b = nrm(ks[17], (L, D), 0.01)
    return {"x": x, "positions": positions, "w_in": w_in, "attn_gain": attn_gain,
            "conv_w": conv_w, "conv_b": conv_b, "conv_ln_g": conv_ln_g, "conv_ln_b": conv_ln_b,
            "w_o": w_o, "ln1_g": ln1_g, "ln1_b": ln1_b, "w_router": w_router, "b_router": b_router,
            "w_gu": w_gu, "b_gu": b_gu, "w_dn": w_dn, "b_dn": b_dn, "ln2_g": ln2_g, "ln2_b": ln2_b}


def reference(x, positions, w_in, attn_gain, conv_w, conv_b, conv_ln_g, conv_ln_b,
              w_o, ln1_g, ln1_b, w_router, b_router, w_gu, b_gu, w_dn, b_dn, ln2_g, ln2_b):
    B, S, D = x.shape
    cos, sin = rotary_tables(positions)
    for l in range(DEPTH):
        h = x @ w_in[l]
        q = h[..., :ATTN_WIDTH].reshape(B, S, ATTN_HEADS, HEAD_DIM)
        k = h[..., ATTN_WIDTH:2 * ATTN_WIDTH].reshape(B, S, ATTN_HEADS, HEAD_DIM)
        v = h[..., 2 * ATTN_WIDTH:3 * ATTN_WIDTH].reshape(B, S, ATTN_HEADS, HEAD_DIM)
        u2 = h[..., 3 * ATTN_WIDTH:]
        q = partial_rotary(q, cos, sin).transpose(0, 2, 1, 3)
        k = partial_rotary(k, cos, sin).transpose(0, 2, 1, 3)
        v = v.transpose(0, 2, 1, 3)
        y_attn = rms_norm(moba_attention(q, k, v), attn_gain[l])
        y_conv = conformer_conv(u2, conv_w[l], conv_b[l], conv_ln_g[l], conv_ln_b[l])
        mix = jnp.concatenate([y_attn, y_conv], axis=-1) @ w_o[l]
        x = layer_norm(DEEPNORM_ALPHA * x + mix, ln1_g[l], ln1_b[l])
        y = moe_ffn(x.reshape(B * S, D), w_router[l], b_router[l], w_gu[l], b_gu[l],
                    w_dn[l], b_dn[l]).reshape(B, S, D)
        x = layer_norm(DEEPNORM_ALPHA * x + y, ln2_g[l], ln2_b[l])
    return x
```

```python
import os
import numpy as np
import concourse.bass as bass
import concourse.mybir as mybir
from concourse.bass_utils import run_bass_kernel_spmd

F32 = mybir.dt.float32
BF16 = mybir.dt.bfloat16
I32 = mybir.dt.int32
U32 = mybir.dt.uint32
AF = mybir.ActivationFunctionType
ALU = mybir.AluOpType
AX = mybir.AxisListType

SEQ = 2048
D = 1024
NE = 32
CAP = 1024
J = CAP // 128
NSB = 2
JS = J // NSB
CAPS = CAP // NSB
ALPHA = float(8.0 ** 0.25)
EPS = 1e-5
NEG = -30000.0
PI = 3.1415925


class Tok:
    __slots__ = ("name", "last_w", "readers", "dsem", "dcount")

    def __init__(self, name):
        self.name = name
        self.last_w = None
        self.readers = {}
        self.dsem = None
        self.dcount = 0


class Op:
    __slots__ = ("eng", "fn", "dma", "deps", "sig", "tok", "has_dep")

    def __init__(self, eng, fn, dma):
        self.eng = eng
        self.fn = fn
        self.dma = dma
        self.deps = ()
        self.sig = None
        self.has_dep = False
        self.tok = None


class Sched:
    ENGS = ("pe", "act", "dve", "pool", "sp")

    def __init__(self, nc):
        self.nc = nc
        self.ops = []
        self.nsem = 0
        self.uid = 0

    def tok(self, name):
        if not hasattr(self, "_tc"):
            self._tc = {}
        if name not in self._tc:
            self._tc[name] = Tok(name)
        return self._tc[name]

    def toks(self, name, n):
        return [self.tok("%s%d" % (name, i)) for i in range(n)]

    def alias(self, new_toks, old_toks):
        olds = {}
        for o in old_toks:
            if o.last_w is not None:
                olds[id(o.last_w)] = o.last_w
            for r in o.readers.values():
                olds[id(r)] = r
        for n in new_toks:
            n.last_w = None
            n.readers = {}
            for r in olds.values():
                self.uid += 1
                n.readers[("al", self.uid)] = r

    def op(self, eng, fn, reads=(), writes=(), dma=False):
        o = Op(eng, fn, dma)
        deps = {}
        for t in reads:
            if t.last_w is not None:
                deps[id(t.last_w)] = t.last_w
        for t in writes:
            if t.last_w is not None:
                deps[id(t.last_w)] = t.last_w
            for r in t.readers.values():
                deps[id(r)] = r
        for t in reads:
            if dma:
                self.uid += 1
                t.readers[("dma", self.uid)] = o
            else:
                t.readers[eng] = o
        for t in writes:
            t.last_w = o
            t.readers = {}
        deps.pop(id(o), None)
        o.deps = list(deps.values())
        for d in o.deps:
            if not (d.eng == "pe" and eng == "pe" and not d.dma):
                d.has_dep = True
        if dma:
            assert len(writes) == 1
            o.tok = writes[0]
        self.ops.append(o)
        return o

    def dma(self, eng, out, in_, reads, write, **kw):
        return self.op(eng, lambda e: e.dma_start(out=out, in_=in_, **kw), reads, [write], dma=True)

    def emit(self, final_toks=()):
        nc = self.nc
        esem = {e: nc.alloc_semaphore("S_" + e) for e in ("pe", "act", "dve", "pool")}
        ecount = {e: 0 for e in esem}
        esem2 = {}
        EPOCH = 8000
        finals = [t.last_w for t in final_toks if t.last_w is not None]
        dma_final = {}
        for f in finals:
            f.has_dep = True
        for o in self.ops:
            if o.dma:
                t = o.tok
                if t.dsem is None:
                    t.dsem = nc.alloc_semaphore("D%d" % self.nsem)
                    self.nsem += 1
                t.dcount += 16
                o.sig = (t.dsem, t.dcount)
                dma_final[id(t.dsem)] = (t.dsem, t.dcount)
            elif o.has_dep:
                ep = ecount[o.eng] // EPOCH
                key = (o.eng, ep)
                if key not in esem2:
                    esem2[key] = esem[o.eng] if ep == 0 else nc.alloc_semaphore("S_%s_%d" % (o.eng, ep))
                o.sig = (esem2[key], ecount[o.eng] % EPOCH + 1)
                ecount[o.eng] += 1
        per = {e: [] for e in self.ENGS}
        for o in self.ops:
            per[o.eng].append(o)
        self.n_waits = 0

        def run(engname, eng):
            waited = {}
            for o in per[engname]:
                for d in o.deps:
                    if d.eng == "pe" and engname == "pe" and not d.dma:
                        continue
                    sem, val = d.sig
                    k = id(sem)
                    if waited.get(k, 0) >= val:
                        continue
                    eng.wait_ge(sem, val)
                    self.n_waits += 1
                    waited[k] = val
                ins = o.fn(eng)
                if o.sig is not None:
                    ins.then_inc(o.sig[0], 16 if o.dma else 1)
            if engname == "sp":
                for d in finals:
                    sem, val = d.sig
                    if waited.get(id(sem), 0) >= val:
                        continue
                    eng.wait_ge(sem, val)
                    waited[id(sem)] = val
                for (sem, val) in dma_final.values():
                    if waited.get(id(sem), 0) >= val:
                        continue
                    eng.wait_ge(sem, val)
                    waited[id(sem)] = val

        with nc.Block() as block:
            block.tensor(lambda e: run("pe", e))
            block.scalar(lambda e: run("act", e))
            block.vector(lambda e: run("dve", e))
            block.gpsimd(lambda e: run("pool", e))
            block.sync(lambda e: run("sp", e))
        self.ecount = ecount
        return {e: len(per[e]) for e in per}


def host_consts():
    c = {}
    ident = np.eye(128, dtype=np.float32)
    p = np.arange(128)
    tri = np.where(p[:, None] > p[None, :], NEG, 0.0).astype(np.float32)
    ustrict = (p[:, None] < p[None, :]).astype(np.float32)
    iota32 = np.tile(np.arange(32, dtype=np.float32)[None, :], (128, 1))
    ecap = iota32 * CAP
    rperm = np.zeros((128, 16), np.float32)
    for m in range(16):
        rperm[(m + 8) % 16, m] = 1.0
    inv_freq = (500000.0 ** (-np.arange(0, 16, 2, dtype=np.float32) / 16)).astype(np.float32)
    misc = np.zeros((128, 16), np.float32)
    for q in range(16):
        misc[q, 0] = inv_freq[q % 8]
        misc[q, 1] = -1.0 if q < 8 else 1.0
    sel = np.zeros((4, 128, 128), np.float32)
    for hh in range(2):
        for m in range(64):
            sel[hh, hh * 64 + m, m] = 1.0
        for m in range(16):
            sel[2 + hh, hh * 64 + (m + 8) % 16, m] = 1.0
    c["cst"] = np.concatenate([ident, tri, ustrict, iota32, ecap, rperm, misc, sel[0], sel[1], sel[2], sel[3]], axis=1).astype(np.float32)
    kaug = np.zeros((8, SEQ), np.float32)
    for n in range(8):
        kaug[n, n * 256:(n + 1) * 256] = 1.0
    c["kaug"] = kaug
    c["tokid"] = (np.arange(16, dtype=np.int32)[None, :] * 128 + p[:, None]).astype(np.int32)
    c["zero_i"] = np.zeros((128, NE * CAP // 128), np.int32)
    return c


CST_COLS = 128 * 3 + 32 + 32 + 16 + 16 + 512


def build(nc, NL, dbg=(), stop=None, NLW=4):
    S = Sched(nc)
    dbg_out = {}

    def din(name, shape, dt=F32):
        return nc.dram_tensor(name, shape, dt, kind="ExternalInput").ap()

    x_d = din("x", [SEQ, D])
    pos_d = din("pos", [1, SEQ], I32)
    w_in_d = din("w_in", [NLW, D, 2560])
    attn_gain_d = din("attn_gain", [NLW, 512])
    conv_w_d = din("conv_w", [NLW, 31, 512])
    conv_b_d = din("conv_b", [NLW, 512])
    conv_ln_g_d = din("conv_ln_g", [NLW, 512])
    conv_ln_b_d = din("conv_ln_b", [NLW, 512])
    w_o_d = din("w_o", [NLW, D, D])
    ln1_g_d = din("ln1_g", [NLW, D])
    ln1_b_d = din("ln1_b", [NLW, D])
    w_router_d = din("w_router", [NLW, D, NE])
    b_router_d = din("b_router", [NLW, NE])
    w_gu_d = din("w_gu", [NLW, NE, D, 2 * D])
    b_gu_d = din("b_gu", [NLW, NE, 2 * D])
    w_dn_d = din("w_dn", [NLW, NE, D, D])
    b_dn_d = din("b_dn", [NLW, NE, D])
    ln2_g_d = din("ln2_g", [NLW, D])
    ln2_b_d = din("ln2_b", [NLW, D])
    cst_d = din("cst", [128, CST_COLS])
    kaug_d = din("kaug", [8, SEQ])
    tokid_d = din("tokid", [128, 16], I32)
    zero_i_d = din("zero_i", [128, NE * CAP // 128], I32)
    out_d = nc.dram_tensor("out", [SEQ, D], F32, kind="ExternalOutput").ap()
    XTM_d = nc.dram_tensor("xtm_scr", [SEQ, D], BF16, kind="Internal").ap()
    S2T_d = nc.dram_tensor("s2t_scr", [NE * CAP, 1], I32, kind="Internal").ap()
    YS_d = nc.dram_tensor("ys_scr", [NE * CAP, D], F32, kind="Internal").ap()

    def sb(name, shape, dt):
        return nc.alloc_sbuf_tensor("sb_" + name, shape, dt)

    H = sb("H", [128, 8, SEQ], BF16)
    L = sb("L", [128, 8, SEQ], BF16)
    tH = S.toks("H", 4)
    tL = S.toks("L", 4)
    cst = sb("cst", [128, CST_COLS], F32)
    ident_f = cst[:, 0:128]
    tri_f = cst[:, 128:256]
    ustrict_f = cst[:, 256:384]
    iota32 = cst[:, 384:416]
    ecap = cst[:, 416:448]
    rperm_f = cst[:, 448:464]
    invf = cst[:, 464:465]
    ssign = cst[:, 465:466]
    cbf = sb("cbf", [128, 128 * 7 + 16 + 512], BF16)
    psel = [cbf[:, 912 + i * 128:912 + (i + 1) * 128] for i in range(4)]
    e0 = sb("e0", [128, 128], F32)
    br_pad = sb("br_pad", [128, NE], F32)
    ident_bf = cbf[:, 0:128]
    tri_bf = cbf[:, 128:256]
    ustrict_bf = cbf[:, 256:384]
    aI_hi = cbf[:, 384:512]
    aI_lo = cbf[:, 512:640]
    ones_bf = cbf[:, 640:768]
    onesD_bf = cbf[:, 768:896]
    rperm_bf = cbf[:, 896:912]
    onesC_bf = sb("onesC", [128, 128], BF16)
    tokid = sb("tokid", [128, 16], I32)
    tabC = sb("tabC", [128, SEQ], F32)
    tabS = sb("tabS", [128, SEQ], F32)
    tC = S.tok("cst")
    tCB = S.tok("cbf")
    tTab = S.tok("tab")
    tTokid = S.tok("tokid")
    pcol = sb("pcol", [128, 48], F32)
    convw = sb("convw", [128, 124], F32)
    bguT = sb("bguT", [128, 16, NE], F32)
    wr_hi = sb("wr_hi", [128, 8, NE], BF16)
    wr_lo = sb("wr_lo", [128, 8, NE], BF16)
    tP = S.tok("params")
    tBgu = S.tok("bgu")
    tWr = S.tok("wr")
    g4 = sb("g4", [128, 16, 4], F32)
    slot_i = sb("slot_i", [128, 16, 4], I32)
    mask_all = sb("mask_all", [128, 16, NE], BF16)
    idx_all = sb("idx_all", [128, NE, J], I32)
    tG4 = S.toks("g4", 16)
    tSlot = S.toks("slot", 16)
    tMask = S.toks("mask", 16)
    tIdx = S.tok("idx_all")

    ARENA = 114 * 1024
    arena = sb("arena", [128, ARENA // 2], BF16)

    def av(off, shape, dt):
        esz = 2 if dt == BF16 else 4
        n = 1
        for s in shape[1:]:
            n *= s
        assert off % 4 == 0 and off + n * esz <= ARENA, (off, shape)
        ap = arena[:, off // 2: off // 2 + n * esz // 2]
        if dt != BF16:
            ap = ap.bitcast(dt)
        if len(shape) == 3:
            ap = ap.rearrange("p (a b) -> p a b", a=shape[1])
        elif len(shape) == 4:
            ap = ap.rearrange("p (a b c) -> p a b c", a=shape[1], b=shape[2])
        return ap

    K = 1024
    mixT = av(0, [128, 8, SEQ], BF16)
    tMix = [[S.tok("mix%d_%d" % (f, tc)) for tc in range(4)] for f in range(8)]
    RING0 = 32 * K
    ring = [av(RING0 + i * 8 * K, [128, 8, 512], BF16) for i in range(6)]
    tRing = S.toks("ring", 6)
    PH0 = 80 * K
    phase_toks = []

    def new_phase(toks):
        nonlocal phase_toks
        S.alias(toks, phase_toks)
        phase_toks = list(toks)

    psum = [nc.alloc_psum_tensor("ps%d" % i, [128, 512], F32) for i in range(8)]
    tPs = S.toks("ps", 8)
    rot = {"big": [0, 0, 1, 2, 3], "acc": [0, 4, 5], "aux": [0, 6, 7]}

    def ps_next(group):
        r = rot[group]
        i = r[1 + r[0] % (len(r) - 1)]
        r[0] += 1
        return psum[i], tPs[i]

    def mm(out, lhsT, rhs, start, stop, reads, writes):
        S.op("pe", lambda e: e.matmul(out, lhsT=lhsT, rhs=rhs, start=start, stop=stop), reads, writes)

    def tr(out, in_, ident, reads, writes):
        S.op("pe", lambda e: e.transpose(out=out, in_=in_, identity=ident), reads, writes)

    def act(out, in_, func, reads, writes, **kw):
        S.op("act", lambda e: e.activation(out=out, in_=in_, func=func, **kw), reads, writes)

    def ts(eng, out, in0, s1, s2, op0, op1, reads, writes):
        if op1 is None:
            S.op(eng, lambda e: e.tensor_scalar(out=out, in0=in0, scalar1=s1, scalar2=None, op0=op0), reads, writes)
        else:
            S.op(eng, lambda e: e.tensor_scalar(out=out, in0=in0, scalar1=s1, scalar2=s2, op0=op0, op1=op1), reads, writes)

    def tt(eng, out, in0, in1, op, reads, writes):
        S.op(eng, lambda e: e.tensor_tensor(out=out, in0=in0, in1=in1, op=op), reads, writes)

    def stt(out, in0, scalar, in1, op0, op1, reads, writes):
        S.op("dve", lambda e: e.scalar_tensor_tensor(out=out, in0=in0, scalar=scalar, in1=in1, op0=op0, op1=op1), reads, writes)

    def cp(eng, out, in_, reads, writes):
        if eng == "act":
            S.op("act", lambda e: e.copy(out=out, in_=in_), reads, writes)
        else:
            S.op(eng, lambda e: e.tensor_copy(out=out, in_=in_), reads, writes)

    def dbg_dump(name, ap_sb, shape, reads):
        if name not in dbg:
            return
        d = nc.dram_tensor("dbg_" + name, shape, F32, kind="ExternalOutput").ap()
        t = S.tok("dbg_" + name)
        S.dma("pool", d, ap_sb, reads, t)
        dbg_out[name] = t

    def tcs(tc):
        return slice(tc * 512, (tc + 1) * 512)

    S.dma("sp", cst[:], cst_d, [], tC)
    S.dma("sp", tokid[:], tokid_d, [], tTokid)
    tS2T = S.toks("s2t", 16)
    for i in range(16):
        pass
    zi = av(PH0, [128, NE * CAP // 128], I32)
    tZi = S.tok("zi")
    S.dma("sp", zi, zero_i_d, [], tZi)
    tS2Tall = S.tok("s2t_init")
    S.dma("sp", S2T_d.rearrange("(p j) o -> p (j o)", p=128), zi, [tZi], tS2Tall)
    for t in tS2T:
        S.alias([t], [tS2Tall])
    cp("dve", cbf[:, 0:384], cst[:, 0:384], [tC], [tCB])
    hi_a = float(np.asarray(ALPHA, np.float32).astype(mybir_bf16()).astype(np.float32))
    lo_a = float(np.float32(ALPHA) - np.float32(hi_a))
    ts("dve", aI_hi, ident_f, hi_a, None, ALU.mult, None, [tC, tCB], [tCB])
    ts("dve", aI_lo, ident_f, lo_a, None, ALU.mult, None, [tC, tCB], [tCB])
    S.op("dve", lambda e: e.memset(ones_bf, 1.0), [tCB], [tCB])
    S.op("dve", lambda e: e.memset(onesD_bf, 1.0 / 1024), [tCB], [tCB])
    S.op("dve", lambda e: e.memset(onesC_bf[:], 1.0 / 512), [tCB], [tCB])
    cp("dve", rperm_bf, rperm_f, [tC, tCB], [tCB])
    cp("dve", cbf[:, 912:912 + 512], cst[:, 480:480 + 512], [tC, tCB], [tCB])
    S.op("dve", lambda e: e.memset(e0[:], 0.0), [tCB], [tCB])
    S.op("dve", lambda e: e.memset(e0[0:1, :], 1.0), [tCB], [tCB])
    S.op("dve", lambda e: e.memset(br_pad[:], 0.0), [tCB], [tWr])

    pos_i = av(0, [128, SEQ], I32)
    ang = av(8 * K, [128, SEQ], F32)
    kf = av(16 * K, [128, SEQ], F32)
    ki = av(24 * K, [128, SEQ], I32)
    tA = S.tok("ang")
    tPi = S.tok("pos_i")
    S.dma("sp", pos_i[0:16, :], pos_d.to_broadcast([16, SEQ]), [], tPi)
    P16 = slice(0, 16)
    cp("dve", ang[P16], pos_i[P16], [tPi], [tA])
    ts("dve", ang[P16], ang[P16], invf[P16], None, ALU.mult, None, [tA, tC], [tA])
    ts("dve", kf[P16], ang[P16], float(1.0 / (2 * np.pi)), None, ALU.mult, None, [tA], [tA])
    cp("dve", ki[P16], kf[P16], [tA], [tA])
    cp("dve", kf[P16], ki[P16], [tA], [tA])
    c1 = float(np.float32(2 * np.pi))
    c2 = float(2 * np.pi - np.float64(np.float32(2 * np.pi)))
    stt(ang[P16], kf[P16], -c1, ang[P16], ALU.mult, ALU.add, [tA], [tA])
    stt(ang[P16], kf[P16], -c2, ang[P16], ALU.mult, ALU.add, [tA], [tA])
    S.op("dve", lambda e: e.memset(tabC[:], 1.0), [], [tTab])
    S.op("dve", lambda e: e.memset(tabS[:], 0.0), [], [tTab])
    for (tab, shift) in ((tabS, 0.0), (tabC, float(np.pi / 2))):
        y = kf
        ts("dve", y[P16], ang[P16], shift, None, ALU.add, None, [tA], [tA])
        corr = ki.bitcast(F32)
        ts("dve", corr[P16], y[P16], float(np.pi), float(-2 * np.pi), ALU.is_gt, ALU.mult, [tA], [tA])
        tt("dve", y[P16], y[P16], corr[P16], ALU.add, [tA], [tA])
        ts("dve", corr[P16], y[P16], float(-np.pi), float(2 * np.pi), ALU.is_lt, ALU.mult, [tA], [tA])
        tt("dve", y[P16], y[P16], corr[P16], ALU.add, [tA], [tA])
        ts("dve", y[P16], y[P16], PI, -PI, ALU.min, ALU.max, [tA], [tA])
        if shift == 0.0:
            act(tab[P16], y[P16], AF.Sin, [tA, tC], [tTab], scale=ssign[P16])
        else:
            act(tab[P16], y[P16], AF.Sin, [tA], [tTab])

    xin = [av(RING0 + i * 4 * K, [128, D], F32) for i in range(2)]
    tXin = S.toks("xin", 2)
    init_toks = [tZi, tA, tPi] + tXin

    def split_hilo(src_f32, c, cols, tcw, reads):
        cp("act", H[:, c, cols], src_f32, reads, [tH[tcw]])
        tt("dve", L[:, c, cols], src_f32, H[:, c, cols], ALU.subtract, reads + [tH[tcw]], [tL[tcw]])

    for tti in range(16):
        xi = xin[tti % 2]
        S.dma("sp", xi, x_d[tti * 128:(tti + 1) * 128, :], [], tXin[tti % 2])
        for half in range(2):
            ps, tps = ps_next("aux")
            for cc in range(4):
                c = half * 4 + cc
                tr(ps[:, cc * 128:(cc + 1) * 128], xi[:, c * 128:(c + 1) * 128], ident_f, [tXin[tti % 2], tC], [tps])
            for cc in range(4):
                c = half * 4 + cc
                split_hilo(ps[:, cc * 128:(cc + 1) * 128], c, slice(tti * 128, (tti + 1) * 128), tti // 4, [tps])
    phase_toks = init_toks
    S.alias(tRing, tXin)

    def ln_fm(z, zb, sd, rstd, tZ, gcol, bcol, tc, n_ch, ones_ap, dst_hl, dst_mix=None, silu=False, tdst=None):
        cp("pool", zb, z, [tZ], [tZ])
        pm, tpm = ps_next("acc")
        for c in range(n_ch):
            mm(pm[:], ones_ap, zb[:, c, :], c == 0, c == n_ch - 1, [tZ, tCB], [tpm])
        for c in range(n_ch):
            tt("dve", z[:, c, :], z[:, c, :], pm[:], ALU.subtract, [tZ, tpm], [tZ])
        act(zb, z, AF.Square, [tZ], [tZ])
        pv, tpv = ps_next("acc")
        for c in range(n_ch):
            mm(pv[:], ones_ap, zb[:, c, :], c == 0, c == n_ch - 1, [tZ, tCB], [tpv])
        act(sd, pv[:], AF.Sqrt, [tpv, tCB], [tZ], bias=epsc[:, 0:1])
        S.op("dve", lambda e: e.reciprocal(out=rstd, in_=sd), [tZ], [tZ])
        for c in range(n_ch):
            tt("dve", z[:, c, :], z[:, c, :], rstd, ALU.mult, [tZ], [tZ])
        for c in range(n_ch):
            if silu:
                act(dst_mix[:, c, :], z[:, c, :], AF.Silu, [tZ, tP], [tdst[c]], scale=pcol[:, gcol + c:gcol + c + 1],
                    bias=pcol[:, bcol + c:bcol + c + 1])
            else:
                ts("pool", z[:, c, :], z[:, c, :], pcol[:, gcol + c:gcol + c + 1], pcol[:, bcol + c:bcol + c + 1], ALU.mult, ALU.add,
                   [tZ, tP], [tZ])
                split_hilo(z[:, c, :], c, tcs(tc), tc, [tZ])

    epsc = sb("epsc", [128, 1], F32)
    S.op("dve", lambda e: e.memset(epsc[:], EPS), [], [tCB])

    order = ['init', 'pre', 'conv', 'att', 'p3', 'moe', 'cmb']
    lim = order.index(stop) if stop else 99
    for l in range(NL if lim > 0 else 0):
        st1 = av(PH0, [128, 128], F32)
        st2 = av(PH0 + 512, [128, 128], F32)
        st3 = av(PH0 + 1 * K, [128, 2 * D], F32)
        wr_f = av(PH0 + 9 * K, [128, 8, NE], F32)
        tSt = S.toks("st", 4)
        S.alias([t for f in range(8) for t in tMix[f]], phase_toks)
        new_phase(tSt)
        vecs = [(ln1_g_d, 8), (ln1_b_d, 8), (ln2_g_d, 8), (ln2_b_d, 8), (conv_b_d, 4), (conv_ln_g_d, 4), (conv_ln_b_d, 4),
                (attn_gain_d, 4)]
        S.op("pool", lambda e: e.memset(st1, 0.0), [], [tSt[0]])
        S.op("pool", lambda e: e.memset(st2, 0.0), [], [tSt[1]])
        S.op("pool", lambda e: e.memset(st3, 0.0), [], [tSt[2]])
        r0 = 0
        for (vd, n) in vecs:
            S.dma("sp", st1[r0:r0 + n, :], vd[l].rearrange("(n p) -> n p", p=128), [], tSt[0])
            r0 += n
        S.dma("sp", st2[0:124, :], conv_w_d[l].rearrange("j (c p) -> (j c) p", p=128), [], tSt[1])
        S.dma("sp", st3[0:NE, :], b_gu_d[l], [], tSt[2])
        S.dma("sp", wr_f, w_router_d[l].rearrange("(c p) e -> p c e", p=128), [], tSt[3])
        S.dma("sp", br_pad[0:1, :], b_router_d[l:l + 1, :], [], tWr)
        ps, tps = ps_next("aux")
        tr(ps[:, 0:128], st1[:, :], ident_f, [tSt[0], tC], [tps])
        cp("dve", pcol[:], ps[:, 0:48], [tps], [tP])
        ps, tps = ps_next("aux")
        tr(ps[:, 0:128], st2[:, :], ident_f, [tSt[1], tC], [tps])
        cp("dve", convw[:], ps[:, 0:124], [tps], [tP])
        ps, tps = ps_next("aux")
        for g in range(4):
            ps, tps = ps_next("aux")
            for c4 in range(4):
                c = g * 4 + c4
                tr(ps[:, c4 * 128:(c4 + 1) * 128], st3[:, c * 128:(c + 1) * 128], ident_f, [tSt[2], tC], [tps])
            cp("dve", bguT[:, g * 4:(g + 1) * 4, :], ps[:].rearrange("p (a b) -> p a b", a=4)[:, :, 0:NE], [tps], [tBgu])
        ts("dve", bguT[:, 8:16, :], bguT[:, 8:16, :], 1.0, None, ALU.add, None, [tBgu], [tBgu])
        cp("dve", wr_hi[:], wr_f, [tSt[3]], [tWr])
        tt("dve", wr_lo[:], wr_f, wr_hi[:], ALU.subtract, [tSt[3], tWr], [tWr])

        if lim < 2:
            break
        u = av(PH0, [128, SEQ + 32], BF16)
        diag = av(PH0 + 5 * K, [128, 31, 128], BF16)
        sg = av(PH0 + 13 * K, [128, 512], F32)
        zc = av(PH0 + 15 * K, [128, 4, 512], F32)
        zcb = av(PH0 + 23 * K, [128, 4, 512], BF16)
        sdc = av(PH0 + 27 * K, [128, 512], F32)
        rsc = av(PH0 + 29 * K, [128, 512], F32)
        tU = S.tok("u")
        tDg = S.tok("diag")
        tSg = S.tok("sg")
        tZc = S.tok("zc")
        new_phase([tU, tDg, tSg, tZc])
        for cc in range(4):
            sl = cc % 2
            wsl = ring[sl]
            S.dma("pool", wsl[:, :, 0:128], w_in_d[l, :, 1536 + cc * 128:1536 + (cc + 1) * 128].rearrange("(c p) n -> p c n", p=128),
                  [], tRing[sl])
            S.dma("pool", wsl[:, :, 128:256], w_in_d[l, :, 2048 + cc * 128:2048 + (cc + 1) * 128].rearrange("(c p) n -> p c n", p=128),
                  [], tRing[sl])
            S.op("dve", lambda e: e.memset(u[:, 0:32], 0.0), [], [tU])
            for j in range(31):
                col = j * 4 + cc
                ts("dve", diag[:, j, :], ident_bf, convw[:, col:col + 1], None, ALU.mult, None, [tCB, tP], [tDg])
            for tc in range(4):
                pa, tpa = ps_next("big")
                pg, tpg = ps_next("big")
                for c in range(8):
                    mm(pa[:], wsl[:, c, 0:128], H[:, c, tcs(tc)], c == 0, c == 7, [tRing[sl], tH[tc]], [tpa])
                for c in range(8):
                    mm(pg[:], wsl[:, c, 128:256], H[:, c, tcs(tc)], c == 0, c == 7, [tRing[sl], tH[tc]], [tpg])
                act(sg, pg[:], AF.Sigmoid, [tpg], [tSg])
                tt("dve", u[:, 32 + tc * 512:32 + (tc + 1) * 512], pa[:], sg, ALU.mult, [tpa, tSg], [tU])
            for tc in range(4):
                pc, tpc = ps_next("big")
                for j in range(31):
                    o0 = 32 + tc * 512 - 30 + j
                    mm(pc[:], diag[:, j, :], u[:, o0:o0 + 512], j == 0, j == 30, [tDg, tU], [tpc])
                act(mixT[:, 4 + cc, tcs(tc)], pc[:], AF.Identity, [tpc, tP], [tMix[4 + cc][tc]], bias=pcol[:, 32 + cc:33 + cc])
        for tc in range(4):
            for cc in range(4):
                cp("dve", zc[:, cc, :], mixT[:, 4 + cc, tcs(tc)], [tMix[4 + cc][tc]], [tZc])
            ln_fm(zc, zcb, sdc, rsc, tZc, 36, 40, tc, 4, onesC_bf[:], None, dst_mix=mixT[:, 4:8, tcs(tc)], silu=True,
                  tdst=[tMix[4 + cc][tc] for cc in range(4)])
        dbg_dump("yconv", mixT[:, 4:8, :], [128, 4, SEQ], [tMix[4 + cc][tc] for cc in range(4) for tc in range(4)])

        if lim < 3:
            break
        qT = av(PH0, [128, SEQ], BF16)
        kT = av(PH0 + 4 * K, [128, SEQ], BF16)
        Vp = av(PH0 + 8 * K, [128, 16, 128], BF16)
        PTs = [av(PH0 + 12 * K + i * K, [128, 512], BF16) for i in range(4)]
        t1 = av(PH0 + 16 * K, [128, 512], F32)
        t2 = av(PH0 + 18 * K, [128, 512], F32)
        G = av(PH0 + 20 * K, [128, 8, 8], F32)
        mx = av(PH0 + 20 * K + 256, [128, 8, 8], F32)
        km = av(PH0 + 20 * K + 512, [128, 8], F32)
        km_bf = av(PH0 + 20 * K + 512 + 32, [128, 8], BF16)
        Bp = av(PH0 + 21 * K, [128, 8, 128], BF16)
        rden = av(PH0 + 23 * K, [128, 512], F32)
        sqa = av(PH0 + 25 * K, [128, 4, 512], BF16)
        sda = av(PH0 + 29 * K, [128, 512], F32)
        rsa = av(PH0 + 31 * K, [128, 512], F32)
        tQ = S.tok("qT")
        tK = S.tok("kT")
        tQa = S.tok("qTaug")
        tV = S.tok("Vp")
        tPT = S.toks("PT", 4)
        tT1 = S.tok("t1")
        tT2 = S.tok("t2")
        tG = S.tok("G")
        tKm = S.tok("km")
        tBp = S.tok("Bp")
        tRd = S.tok("rden")
        tSq = S.tok("sqa")
        new_phase([tQ, tK, tQa, tV, tT1, tT2, tG, tKm, tBp, tRd, tSq] + tPT)
        S.op("pool", lambda e: e.memset(kT[64:128, :], 0.0), [], [tK])
        S.dma("pool", kT[64:72, :], kaug_d, [], tK)
        S.op("pool", lambda e: e.memset(qT[64:128, :], 0.0), [], [tQa])
        S.op("pool", lambda e: e.memset(km_bf, 0.0), [], [tKm])
        qpair = av(PH0 + 33 * K, [128, 512], BF16)
        wqk_f = av(RING0 + 4 * 8 * K, [128, 8, 256], F32)
        wlo = av(RING0 + 5 * 8 * K, [128, 8, 256], BF16)
        qpair_lo = av(RING0 + 5 * 8 * K + 4 * K, [128, 512], BF16)
        qlo = av(RING0 + 5 * 8 * K + 5 * K, [128, 1024], BF16)
        km_lo = av(RING0 + 5 * 8 * K + 7 * K, [128, 8], BF16)
        tQlo = S.tok("qlo")
        S.alias([tQlo], [tRing[5]])
        S.op("pool", lambda e: e.memset(qlo, 0.0), [], [tQlo])
        S.op("pool", lambda e: e.memset(km_lo, 0.0), [tKm], [tKm])
        tQp = S.tok("qpair")
        S.alias([tQp], [tSq])
        S.op("pool", lambda e: e.memset(Bp, 0.0), [], [tBp])
        PTn = [0]
        AL = int(os.environ.get('ATT_LIM', '9'))
        for jp in range(4 if AL > 1 else 0):
            sl = jp % 2
            wsl = ring[sl]
            for part in range(3):
                S.dma("pool", wsl[:, :, part * 128:(part + 1) * 128],
                      w_in_d[l, :, part * 512 + jp * 128:part * 512 + (jp + 1) * 128].rearrange("(c p) n -> p c n", p=128),
                      [], tRing[sl])
            for part in range(2):
                S.dma("sp", wqk_f[:, :, part * 128:(part + 1) * 128],
                      w_in_d[l, :, part * 512 + jp * 128:part * 512 + (jp + 1) * 128].rearrange("(c p) n -> p c n", p=128),
                      [], tRing[4])
            tt("dve", wlo, wqk_f, wsl[:, :, 0:256], ALU.subtract, [tRing[4], tRing[sl]], [tRing[5]])
            for g4i in range(4):
                ps, tps = ps_next("big")
                for kk in range(4):
                    kt = g4i * 4 + kk
                    for c in range(8):
                        mm(ps[:, kk * 128:(kk + 1) * 128], H[:, c, kt * 128:(kt + 1) * 128], wsl[:, c, 256:384], c == 0, c == 7,
                           [tH[kt // 4], tRing[sl]], [tps])
                cp("act", Vp[:, g4i * 4:(g4i + 1) * 4, :].rearrange("p a b -> p (a b)"), ps[:], [tps], [tV])
            for hh in range(2 if AL > 2 else 0):
                h = 2 * jp + hh
                for (dst, tD, col0) in ((kT, tK, 128), (qT, tQ, 0)):
                    for tc in range(4):
                        ps, tps = ps_next("big")
                        for c in range(8):
                            mm(ps[:], wsl[:, c, col0:col0 + 128], H[:, c, tcs(tc)], c == 0, False, [tRing[sl], tH[tc]], [tps])
                            mm(ps[:], wsl[:, c, col0:col0 + 128], L[:, c, tcs(tc)], False, False, [tRing[sl], tL[tc]], [tps])
                            mm(ps[:], wlo[:, c, col0:col0 + 128], H[:, c, tcs(tc)], False, c == 7, [tRing[5], tH[tc]], [tps])
                        cp("act", qpair, ps[:], [tps], [tQp])
                        tt("dve", qpair_lo, ps[:], qpair, ALU.subtract, [tps, tQp], [tQp])
                        pa_, tpa_ = ps_next("big")
                        mm(pa_[:], psel[hh], qpair, True, False, [tQp, tCB], [tpa_])
                        mm(pa_[:], psel[hh], qpair_lo, False, True, [tQp, tCB], [tpa_])
                        pr, tpr = ps_next("aux")
                        mm(pr[:], psel[2 + hh], qpair, True, False, [tQp, tCB], [tpr])
                        mm(pr[:], psel[2 + hh], qpair_lo, False, True, [tQp, tCB], [tpr])
                        tt("dve", t1[0:64, :], pa_[0:64, :], tabC[0:64, tcs(tc)], ALU.mult, [tpa_, tTab], [tT1])
                        tt("dve", t2[0:64, :], pr[0:64, :], tabS[0:64, tcs(tc)], ALU.mult, [tpr, tTab], [tT2])
                        tt("dve", t1[0:64, :], t1[0:64, :], t2[0:64, :], ALU.add, [tT1, tT2], [tT1])
                        cp("act", dst[0:64, tcs(tc)], t1[0:64, :], [tT1], [tD])
                        if dst is qT and tc >= 2:
                            tt("dve", qlo[0:64, (tc - 2) * 512:(tc - 1) * 512], t1[0:64, :], dst[0:64, tcs(tc)], ALU.subtract,
                               [tT1, tD], [tQlo])
                        if dst is kT:
                            S.op("dve", lambda e, tc=tc: e.tensor_reduce(out=km[0:64, tc * 2:tc * 2 + 2],
                                                                         in_=t1[0:64, :].rearrange("p (n k) -> p n k", k=256),
                                                                         axis=AX.X, op=ALU.add), [tT1], [tKm])
                if "q" in dbg and l == 0 and h == 1:
                    dbg_dump("q", qT[0:64, :], [64, SEQ], [tQ])
                    dbg_dump("k", kT[0:64, :], [64, SEQ], [tK])
                if AL < 4:
                    continue
                ts("dve", km[0:64, :], km[0:64, :], 1.0 / 256, None, ALU.mult, None, [tKm], [tKm])
                cp("dve", km_bf[0:64, :], km[0:64, :], [tKm], [tKm])
                tt("dve", km_lo[0:64, :], km[0:64, :], km_bf[0:64, :], ALU.subtract, [tKm], [tKm])
                pg, tpg = ps_next("aux")
                for qi in range(8):
                    qt = 8 + qi
                    mm(pg[:, qi * 8:(qi + 1) * 8], qT[:, qt * 128:(qt + 1) * 128], km_bf[:, :], True, False, [tQ, tQa, tKm], [tpg])
                    mm(pg[:, qi * 8:(qi + 1) * 8], qlo[:, qi * 128:(qi + 1) * 128], km_bf[:, :], False, False, [tQlo, tKm], [tpg])
                    mm(pg[:, qi * 8:(qi + 1) * 8], qT[:, qt * 128:(qt + 1) * 128], km_lo[:, :], False, True, [tQ, tQa, tKm], [tpg])
                cp("dve", G.rearrange("p a b -> p (a b)"), pg[:, 0:64], [tpg], [tG])
                for own in range(4, 8):
                    qi0 = (own - 4) * 2
                    S.op("dve", lambda e, qi0=qi0, own=own: e.memset(G[:, qi0:qi0 + 2, own:8], -1e30), [tG], [tG])
                for qi in range(8):
                    own = (8 + qi) // 2
                    S.op("dve", lambda e, qi=qi: e.max(out=mx[:, qi, :], in_=G[:, qi, :]), [tG], [tG])
                    ts("dve", Bp[:, qi, 64:64 + own], G[:, qi, 0:own], mx[:, qi, 2:3], NEG, ALU.is_lt, ALU.mult, [tG], [tBp])
                if AL < 5:
                    continue
                for half in range(2):
                    pb, tpb = ps_next("aux")
                    for i in range(4):
                        qi = half * 4 + i
                        mm(pb[:, i * 128:(i + 1) * 128], Bp[:, qi, :], ident_bf, True, True, [tBp, tCB], [tpb])
                    cp("dve", qT[64:128, 1024 + half * 512:1024 + (half + 1) * 512], pb[64:128, :], [tpb], [tQa])
                if AL < 6:
                    continue
                rows = slice(hh * 64, (hh + 1) * 64)
                for c in range(4):
                    nkt = 4 * c + 4
                    po, tpo = psum[4], tPs[4]
                    pd, tpd = psum[5], tPs[5]
                    for kt in range(nkt):
                        q0 = max(kt * 128, c * 512)
                        off = q0 - c * 512
                        pss, tpss = ps_next("big")
                        diagt = kt >= 4 * c
                        mm(pss[:, off:512], kT[:, kt * 128:(kt + 1) * 128], qT[:, q0:(c + 1) * 512], True, not diagt,
                           [tK, tQ, tQa], [tpss])
                        if diagt:
                            mm(pss[:, off:off + 128], ident_bf, tri_bf, False, True, [tCB], [tpss])
                        pi = PTn[0] % 4
                        PTn[0] += 1
                        act(PTs[pi][:, off:512], pss[:, off:512], AF.Exp, [tpss], [tPT[pi]], scale=0.125)
                        mm(po[:, off:512], Vp[:, kt, :], PTs[pi][:, off:512], kt == 0, kt == nkt - 1,
                           [tV, tPT[pi]], [tpo])
                        mm(pd[:, off:512], ones_bf, PTs[pi][:, off:512], kt == 0, kt == nkt - 1, [tCB, tPT[pi]], [tpd])
                    S.op("dve", lambda e, rows=rows, pd=pd: e.reciprocal(out=rden[rows, :], in_=pd[rows, :]), [tpd], [tRd])
                    tt("dve", mixT[rows, jp, tcs(c)], po[rows, :], rden[rows, :], ALU.mult, [tpo, tRd], [tMix[jp][c]])
        dbg_dump("att", mixT[:, 0:4, :], [128, 4, SEQ], [tMix[f][tc] for f in range(4) for tc in range(4)])
        for tc in range(4 if AL >= 6 else 0):
            rd4 = [tMix[f][tc] for f in range(4)]
            act(sqa, mixT[:, 0:4, tcs(tc)], AF.Square, rd4, [tSq])
            pv, tpv = ps_next("acc")
            for f in range(4):
                mm(pv[:], onesC_bf[:], sqa[:, f, :], f == 0, f == 3, [tSq, tCB], [tpv])
            act(sda, pv[:], AF.Sqrt, [tpv], [tSq], bias=epsc[:, 0:1])
            S.op("dve", lambda e: e.reciprocal(out=rsa, in_=sda), [tSq], [tSq])
            for f in range(4):
                stt(mixT[:, f, tcs(tc)], mixT[:, f, tcs(tc)], pcol[:, 44 + f:45 + f], rsa, ALU.mult, ALU.mult, [tSq, tP, tMix[f][tc]],
                    [tMix[f][tc]])
        dbg_dump("yattn", mixT[:, 0:4, :], [128, 4, SEQ], [tMix[f][tc] for f in range(4) for tc in range(4)])

        if lim < 4:
            break
        z = av(PH0, [128, 8, 512], F32)
        zb = av(PH0 + 16 * K, [128, 8, 512], BF16)
        sd = av(PH0 + 24 * K, [128, 512], F32)
        rstd = av(PH0 + 26 * K, [128, 512], F32)
        tZ = S.tok("z")
        new_phase([tZ])
        for half in range(2):
            S.dma("pool", ring[2 + half], w_o_d[l, :, half * 512:(half + 1) * 512].rearrange("(c p) n -> p c n", p=128), [], tRing[2 + half])
        for tc in range(4):
            for cch in range(8):
                ps, tps = ps_next("big")
                wsl = ring[2 + cch // 4]
                tw = tRing[2 + cch // 4]
                cs = slice((cch % 4) * 128, (cch % 4 + 1) * 128)
                for f in range(8):
                    mm(ps[:], wsl[:, f, cs], mixT[:, f, tcs(tc)], f == 0, False, [tw, tMix[f][tc]], [tps])
                mm(ps[:], aI_hi, H[:, cch, tcs(tc)], False, False, [tCB, tH[tc]], [tps])
                mm(ps[:], aI_hi, L[:, cch, tcs(tc)], False, False, [tCB, tL[tc]], [tps])
                mm(ps[:], aI_lo, H[:, cch, tcs(tc)], False, True, [tCB, tH[tc]], [tps])
                cp("act", z[:, cch, :], ps[:], [tps], [tZ])
            ln_fm(z, zb, sd, rstd, tZ, 0, 8, tc, 8, onesD_bf, True)
        if "x1" in dbg and l == 0:
            dbg_dump("x1h", H[:], [128, 8, SEQ], tH)
            dbg_dump("x1l", L[:], [128, 8, SEQ], tL)

        if lim < 5:
            break
        xtm = [av(0 + i * 2 * K, [128, D], BF16) for i in range(2)]
        lg = av(4 * K, [128, NE], F32)
        mx8 = av(4 * K + 128, [128, 8], F32)
        idx8 = av(4 * K + 160, [128, 8], U32)
        nm = av(4 * K + 192, [128, 1], F32)
        e4 = av(4 * K + 196, [128, 4], F32)
        se = av(4 * K + 212, [128, 1], F32)
        rs = av(4 * K + 216, [128, 1], F32)
        idxf = av(4 * K + 220, [128, 4], F32)
        slotall = av(4 * K + 256, [128, NE], F32)
        oh = av(4 * K + 384, [128, NE], F32)
        slotf = av(4 * K + 512, [128, 4], F32)
        xg = [av(5 * K + i * 8 * K, [128, JS, D], BF16) for i in range(2)]
        xgT = av(21 * K, [128, 8, CAPS], BF16)
        hT = av(PH0 + 26 * K, [128, 8, CAPS], BF16)
        gcl = av(PH0, [128, CAPS], F32)
        gs = av(PH0 + 2 * K, [128, CAPS], F32)
        Al = av(PH0 + 4 * K, [128, CAPS], F32)
        y_sb = av(PH0 + 6 * K, [128, JS, D], F32)
        bdn = [av(PH0 + 22 * K, [128, D], F32) for i in range(2)]
        tXtm = S.toks("xtm", 2)
        tXTMd = S.toks("xtmd", 2)
        tR = S.tok("router")
        tXg = S.toks("xg", 2)
        tXgT = S.tok("xgT")
        tHT = S.tok("hT")
        tAct = S.toks("actscr", 3)
        tYsb = S.tok("ysb")
        tBdn = [S.tok("bdn0"), S.tok("bdn0")]
        tYS = S.toks("YS", 8)
        moe_toks = tXtm + [tR] + tXg + [tXgT, tHT] + tAct + [tYsb] + tBdn
        S.alias(moe_toks, phase_toks + [t for f in range(8) for t in tMix[f]])
        phase_toks = list(moe_toks)
        for tti in range(16):
            tc = tti // 4
            tsl = slice(tti * 128, (tti + 1) * 128)
            ps, tps = ps_next("aux")
            psb = ps[:].bitcast(BF16)
            for c in range(8):
                tr(psb[:, c * 128:(c + 1) * 128], H[:, c, tsl], ident_bf, [tH[tc], tCB], [tps])
            xi = tti % 2
            cp("act", xtm[xi], psb, [tps], [tXtm[xi]])
            S.dma("sp", XTM_d[tsl, :], xtm[xi], [tXtm[xi]], tXTMd[xi])
            pl, tpl = ps_next("aux")
            first = True
            for c in range(8):
                for (a, b, ta) in ((H, wr_hi, tH[tc]), (L, wr_hi, tL[tc]), (H, wr_lo, tH[tc])):
                    mm(pl[:, 0:NE], a[:, c, tsl], b[:, c, :], first, False, [ta, tWr], [tpl])
                    first = False
            mm(pl[:, 0:NE], e0[:], br_pad[:], False, True, [tCB, tWr], [tpl])
            cp("dve", lg, pl[:, 0:NE], [tpl], [tR])
            S.op("dve", lambda e: e.max(out=mx8, in_=lg), [tR], [tR])
            S.op("dve", lambda e: e.max_index(out=idx8, in_max=mx8, in_values=lg), [tR], [tR])
            ts("dve", mask_all[:, tti, :], lg, mx8[:, 3:4], None, ALU.is_ge, None, [tR], [tMask[tti]])
            ts("dve", nm, mx8[:, 0:1], -1.0, None, ALU.mult, None, [tR], [tR])
            act(e4, mx8[:, 0:4], AF.Exp, [tR], [tR], bias=nm, accum_out=se)
            S.op("dve", lambda e: e.reciprocal(out=rs, in_=se), [tR], [tR])
            ts("dve", g4[:, tti, :], e4, rs, None, ALU.mult, None, [tR], [tG4[tti]])
            cp("dve", idxf, idx8[:, 0:4], [tR], [tR])
            pr, tpr = ps_next("aux")
            for t2i in range(tti):
                mm(pr[:, 0:NE], ones_bf, mask_all[:, t2i, :], t2i == 0, False, [tCB, tMask[t2i]], [tpr])
            mm(pr[:, 0:NE], ustrict_bf, mask_all[:, tti, :], tti == 0, True, [tCB, tMask[tti]], [tpr])
            tt("dve", slotall, pr[:, 0:NE], ecap, ALU.add, [tpr, tC], [tR])
            for k in range(4):
                ts("dve", oh, iota32, idxf[:, k:k + 1], None, ALU.is_equal, None, [tR, tC], [tR])
                tt("dve", oh, oh, slotall, ALU.mult, [tR], [tR])
                S.op("dve", lambda e, k=k: e.reduce_sum(out=slotf[:, k:k + 1], in_=oh, axis=AX.X), [tR], [tR])
            cp("dve", slot_i[:, tti, :], slotf, [tR], [tSlot[tti]])
            for k in range(4):
                S.op("pool", lambda e, tti=tti, k=k: e.indirect_dma_start(
                    out=S2T_d, out_offset=bass.IndirectOffsetOnAxis(ap=slot_i[:, tti, k:k + 1], axis=0),
                    in_=tokid[:, tti:tti + 1], in_offset=None), [tSlot[tti], tTokid], [tS2T[tti]], dma=True)
        S.dma("sp", idx_all[:], S2T_d.rearrange("(e p j) o -> p e (j o)", p=128, j=J), tS2T, tIdx)
        if "slot" in dbg and l == 0:
            dbg_dump("slot", slot_i[:].rearrange("p a b -> p (a b)"), [128, 64], tSlot)
            dbg_dump("g4", g4[:].rearrange("p a b -> p (a b)"), [128, 64], tG4)

        def load_w(e, s):
            if e >= NE:
                return
            if s < 4:
                src = w_gu_d[l, e, :, s * 512:(s + 1) * 512]
            else:
                src = w_dn_d[l, e, :, (s - 4) * 512:(s - 3) * 512]
            S.dma("pool", ring[s], src.rearrange("(c p) n -> p c n", p=128), [], tRing[s])

        def gather(un):
            e, sbi = un // NSB, un % NSB
            if e >= NE:
                return
            for j in range(JS):
                S.op("pool", lambda en, e=e, j=j, un=un, sbi=sbi: en.indirect_dma_start(
                    out=xg[un % 2][:, j, :], out_offset=None, in_=XTM_d,
                    in_offset=bass.IndirectOffsetOnAxis(ap=idx_all[:, e, sbi * JS + j:sbi * JS + j + 1], axis=0)),
                    [tIdx] + tXTMd, [tXg[un % 2]], dma=True)
            if sbi == 0:
                S.dma("sp", bdn[0], b_dn_d[l, e:e + 1, :].to_broadcast([128, D]), [], tBdn[0])

        gather(0)
        for s_ in range(6):
            load_w(0, s_)
        for e in range(NE):
            for sbi in range(NSB):
                un = e * NSB + sbi
                last = sbi == NSB - 1
                gather(un + 1)
                for j in range(JS):
                    ps, tps = ps_next("aux")
                    psb = ps[:].bitcast(BF16)
                    for c in range(8):
                        tr(psb[:, c * 128:(c + 1) * 128], xg[un % 2][:, j, c * 128:(c + 1) * 128], ident_bf, [tXg[un % 2], tCB], [tps])
                    cp("act", xgT[:, :, j * 128:(j + 1) * 128], psb.rearrange("p (c t) -> p c t", c=8), [tps], [tXgT])
                for fch in range(8):
                    pg, tpg = ps_next("big")
                    pl, tpl = ps_next("big")
                    sg_, sl_ = fch // 4, 2 + fch // 4
                    fc = slice((fch % 4) * 128, (fch % 4 + 1) * 128)
                    for c in range(8):
                        mm(pg[:, 0:CAPS], ring[sg_][:, c, fc], xgT[:, c, :], c == 0, c == 7, [tRing[sg_], tXgT], [tpg])
                    for c in range(8):
                        mm(pl[:, 0:CAPS], ring[sl_][:, c, fc], xgT[:, c, :], c == 0, c == 7, [tRing[sl_], tXgT], [tpl])
                    ts("dve", gcl, pg[:, 0:CAPS], bguT[:, fch, e:e + 1], 7.0, ALU.add, ALU.min, [tpg, tBgu], [tAct[0]])
                    act(gs, gcl, AF.Gelu_apprx_sigmoid, [tAct[0]], [tAct[1]])
                    ts("dve", Al, pl[:, 0:CAPS], bguT[:, 8 + fch, e:e + 1], 8.0, ALU.add, ALU.min, [tpl, tBgu], [tAct[2]])
                    stt(hT[:, fch, :], Al, -6.0, gs, ALU.max, ALU.mult, [tAct[2], tAct[1]], [tHT])
                    if last and fch == 3:
                        load_w(e + 1, 0)
                        load_w(e + 1, 2)
                    if last and fch == 7:
                        load_w(e + 1, 1)
                        load_w(e + 1, 3)
                for half in range(2):
                    for j in range(JS):
                        ps, tps = ps_next("big")
                        for f in range(8):
                            mm(ps[:], hT[:, f, j * 128:(j + 1) * 128], ring[4 + half][:, f, :], f == 0, f == 7, [tHT, tRing[4 + half]], [tps])
                        tt("dve", y_sb[:, j, half * 512:(half + 1) * 512], ps[:], bdn[0][:, half * 512:(half + 1) * 512], ALU.add,
                           [tps, tBdn[0]], [tYsb])
                    if last:
                        load_w(e + 1, 4 + half)
                S.dma("sp", YS_d[e * CAP:(e + 1) * CAP, :].rearrange("(p j) d -> p j d", j=J)[:, sbi * JS:(sbi + 1) * JS, :], y_sb, [tYsb],
                      tYS[un % 8])

        Yk = [av(i * 4 * K, [128, D], F32) for i in range(4)]
        accs = av(16 * K, [128, D], F32)
        z2 = av(PH0, [128, 8, 512], F32)
        zb2 = av(PH0 + 16 * K, [128, 8, 512], BF16)
        sd2 = av(PH0 + 24 * K, [128, 512], F32)
        rstd2 = av(PH0 + 26 * K, [128, 512], F32)
        tYk = S.toks("Yk", 4)
        tAcc = S.tok("acc")
        tZ2 = S.tok("z2")
        new_phase(tYk + [tAcc, tZ2])
        for tti in range(16):
            tc = tti // 4
            tsl = slice(tti * 128, (tti + 1) * 128)
            for k in range(4):
                S.op("pool", lambda en, tti=tti, k=k: en.indirect_dma_start(
                    out=Yk[k], out_offset=None, in_=YS_d,
                    in_offset=bass.IndirectOffsetOnAxis(ap=slot_i[:, tti, k:k + 1], axis=0)),
                    [tSlot[tti]] + tYS, [tYk[k]], dma=True)
            ts("dve", accs, Yk[0], g4[:, tti, 0:1], None, ALU.mult, None, [tYk[0], tG4[tti]], [tAcc])
            for k in range(1, 4):
                stt(accs, Yk[k], g4[:, tti, k:k + 1], accs, ALU.mult, ALU.add, [tYk[k], tG4[tti], tAcc], [tAcc])
            for half in range(2):
                ps, tps = ps_next("big")
                for cc in range(4):
                    c = half * 4 + cc
                    o = ps[:, cc * 128:(cc + 1) * 128]
                    mm(o, accs[:, c * 128:(c + 1) * 128], ident_f, True, False, [tAcc, tC], [tps])
                    mm(o, aI_hi, H[:, c, tsl], False, False, [tCB, tH[tc]], [tps])
                    mm(o, aI_hi, L[:, c, tsl], False, False, [tCB, tL[tc]], [tps])
                    mm(o, aI_lo, H[:, c, tsl], False, True, [tCB, tH[tc]], [tps])
                cp("act", z2[:, half * 4:(half + 1) * 4, (tti % 4) * 128:(tti % 4 + 1) * 128],
                   ps[:].rearrange("p (c t) -> p c t", c=4), [tps], [tZ2])
            if tti % 4 == 3:
                ln_fm(z2, zb2, sd2, rstd2, tZ2, 16, 24, tc, 8, onesD_bf, True)
        if "x2" in dbg and l == 0:
            dbg_dump("x2h", H[:], [128, 8, SEQ], tH)
            dbg_dump("x2l", L[:], [128, 8, SEQ], tL)

    ob = [av(i * 4 * K, [128, D], F32) for i in range(2)]
    tOb = S.toks("ob", 2)
    tOut = S.toks("out", 2)
    S.alias(tOb, phase_toks + [t for f in range(8) for t in tMix[f]])
    for tti in range(16):
        tc = tti // 4
        tsl = slice(tti * 128, (tti + 1) * 128)
        oi = tti % 2
        for half in range(2):
            ps, tps = ps_next("big")
            for cc in range(4):
                c = half * 4 + cc
                o = ps[:, cc * 128:(cc + 1) * 128]
                mm(o, H[:, c, tsl], ident_bf, True, False, [tH[tc], tCB], [tps])
                mm(o, L[:, c, tsl], ident_bf, False, True, [tL[tc], tCB], [tps])
            cp("act", ob[oi][:, half * 512:(half + 1) * 512], ps[:], [tps], [tOb[oi]])
        S.dma("sp", out_d[tsl, :], ob[oi], [tOb[oi]], tOut[oi])
    counts = S.emit(final_toks=tOut + list(dbg_out.values()))
    return counts, S


def mybir_bf16():
    import ml_dtypes
    return ml_dtypes.bfloat16


_CACHE = {}


def kernel(x, positions, w_in, attn_gain, conv_w, conv_b, conv_ln_g, conv_ln_b, w_o, ln1_g, ln1_b, w_router, b_router,
           w_gu, b_gu, w_dn, b_dn, ln2_g, ln2_b, _nl=4, _dbg=(), _cores=8, _stop=None):
    nc = bass.Bass("TRN2", target_bir_lowering=False)
    counts, S = build(nc, _nl, _dbg, _stop, _nl)
    hc = host_consts()
    f = lambda a: np.ascontiguousarray(np.asarray(a, dtype=np.float32)[:_nl])
    shared = dict(w_in=f(w_in), attn_gain=f(attn_gain), conv_w=f(conv_w), conv_b=f(conv_b), conv_ln_g=f(conv_ln_g),
                  conv_ln_b=f(conv_ln_b), w_o=f(w_o), ln1_g=f(ln1_g), ln1_b=f(ln1_b), w_router=f(w_router), b_router=f(b_router),
                  w_gu=f(w_gu), b_gu=f(b_gu), w_dn=f(w_dn), b_dn=f(b_dn), ln2_g=f(ln2_g), ln2_b=f(ln2_b),
                  cst=hc["cst"], kaug=hc["kaug"], tokid=hc["tokid"], zero_i=hc["zero_i"])
    x = np.asarray(x, dtype=np.float32)
    positions = np.asarray(positions, dtype=np.int32)
    in_maps = []
    for b in range(_cores):
        m = dict(shared)
        m["x"] = np.ascontiguousarray(x[b])
        m["pos"] = np.ascontiguousarray(positions[b:b + 1])
        in_maps.append(m)
    res = run_bass_kernel_spmd(nc, in_maps, core_ids=list(range(_cores)))
    if _dbg:
        return res.results
    return np.stack([r["out"] for r in res.results], axis=0).astype(np.float32)
```

```python
import os
import numpy as np
import concourse.bass as bass
import concourse.mybir as mybir
from concourse.bass_utils import run_bass_kernel_spmd

F32 = mybir.dt.float32
BF16 = mybir.dt.bfloat16
I32 = mybir.dt.int32
U32 = mybir.dt.uint32
AF = mybir.ActivationFunctionType
ALU = mybir.AluOpType
AX = mybir.AxisListType

SEQ = 2048
D = 1024
NE = 32
CAP = 768
J = CAP // 128
NSB = 2
JS = J // NSB
CAPS = CAP // NSB
ALPHA = float(8.0 ** 0.25)
EPS = 1e-5
NEG = -30000.0
PI = 3.1415925


class Tok:
    __slots__ = ("name", "last_w", "readers", "dsem", "dcount")

    def __init__(self, name):
        self.name = name
        self.last_w = None
        self.readers = {}
        self.dsem = None
        self.dcount = 0


class Op:
    __slots__ = ("eng", "fn", "dma", "deps", "sig", "tok", "has_dep")

    def __init__(self, eng, fn, dma):
        self.eng = eng
        self.fn = fn
        self.dma = dma
        self.deps = ()
        self.sig = None
        self.has_dep = False
        self.tok = None


class Sched:
    ENGS = ("pe", "act", "dve", "pool", "sp")

    def __init__(self, nc):
        self.nc = nc
        self.ops = []
        self.nsem = 0
        self.uid = 0

    def tok(self, name):
        if not hasattr(self, "_tc"):
            self._tc = {}
        if name not in self._tc:
            self._tc[name] = Tok(name)
        return self._tc[name]

    def toks(self, name, n):
        return [self.tok("%s%d" % (name, i)) for i in range(n)]

    def alias(self, new_toks, old_toks):
        olds = {}
        for o in old_toks:
            if o.last_w is not None:
                olds[id(o.last_w)] = o.last_w
            for r in o.readers.values():
                olds[id(r)] = r
        for n in new_toks:
            n.last_w = None
            n.readers = {}
            for r in olds.values():
                self.uid += 1
                n.readers[("al", self.uid)] = r

    def op(self, eng, fn, reads=(), writes=(), dma=False):
        o = Op(eng, fn, dma)
        deps = {}
        for t in reads:
            if t.last_w is not None:
                deps[id(t.last_w)] = t.last_w
        for t in writes:
            if t.last_w is not None:
                deps[id(t.last_w)] = t.last_w
            for r in t.readers.values():
                deps[id(r)] = r
        for t in reads:
            if dma:
                self.uid += 1
                t.readers[("dma", self.uid)] = o
            else:
                t.readers[eng] = o
        for t in writes:
            t.last_w = o
            t.readers = {}
        deps.pop(id(o), None)
        o.deps = list(deps.values())
        for d in o.deps:
            if not (d.eng == "pe" and eng == "pe" and not d.dma):
                d.has_dep = True
        if dma:
            assert len(writes) == 1
            o.tok = writes[0]
        self.ops.append(o)
        return o

    def dma(self, eng, out, in_, reads, write, **kw):
        return self.op(eng, lambda e: e.dma_start(out=out, in_=in_, **kw), reads, [write], dma=True)

    def emit(self, final_toks=()):
        nc = self.nc
        esem = {e: nc.alloc_semaphore("S_" + e) for e in ("pe", "act", "dve", "pool")}
        ecount = {e: 0 for e in esem}
        esem2 = {}
        EPOCH = 8000
        finals = [t.last_w for t in final_toks if t.last_w is not None]
        dma_final = {}
        for f in finals:
            f.has_dep = True
        for o in self.ops:
            if o.dma:
                t = o.tok
                if t.dsem is None:
                    t.dsem = nc.alloc_semaphore("D%d" % self.nsem)
                    self.nsem += 1
                t.dcount += 16
                o.sig = (t.dsem, t.dcount)
                dma_final[id(t.dsem)] = (t.dsem, t.dcount)
            elif o.has_dep:
                ep = ecount[o.eng] // EPOCH
                key = (o.eng, ep)
                if key not in esem2:
                    esem2[key] = esem[o.eng] if ep == 0 else nc.alloc_semaphore("S_%s_%d" % (o.eng, ep))
                o.sig = (esem2[key], ecount[o.eng] % EPOCH + 1)
                ecount[o.eng] += 1
        per = {e: [] for e in self.ENGS}
        for o in self.ops:
            per[o.eng].append(o)
        self.n_waits = 0

        def run(engname, eng):
            waited = {}
            for o in per[engname]:
                for d in o.deps:
                    if d.eng == "pe" and engname == "pe" and not d.dma:
                        continue
                    sem, val = d.sig
                    k = id(sem)
                    if waited.get(k, 0) >= val:
                        continue
                    eng.wait_ge(sem, val)
                    self.n_waits += 1
                    waited[k] = val
                ins = o.fn(eng)
                if o.sig is not None:
                    ins.then_inc(o.sig[0], 16 if o.dma else 1)
            if engname == "sp":
                for d in finals:
                    sem, val = d.sig
                    if waited.get(id(sem), 0) >= val:
                        continue
                    eng.wait_ge(sem, val)
                    waited[id(sem)] = val
                for (sem, val) in dma_final.values():
                    if waited.get(id(sem), 0) >= val:
                        continue
                    eng.wait_ge(sem, val)
                    waited[id(sem)] = val

        with nc.Block() as block:
            block.tensor(lambda e: run("pe", e))
            block.scalar(lambda e: run("act", e))
            block.vector(lambda e: run("dve", e))
            block.gpsimd(lambda e: run("pool", e))
            block.sync(lambda e: run("sp", e))
        self.ecount = ecount
        return {e: len(per[e]) for e in per}


def host_consts():
    c = {}
    ident = np.eye(128, dtype=np.float32)
    p = np.arange(128)
    tri = np.where(p[:, None] > p[None, :], NEG, 0.0).astype(np.float32)
    ustrict = (p[:, None] < p[None, :]).astype(np.float32)
    iota32 = np.tile(np.arange(32, dtype=np.float32)[None, :], (128, 1))
    ecap = iota32 * CAP
    rperm = np.zeros((128, 16), np.float32)
    for m in range(16):
        rperm[(m + 8) % 16, m] = 1.0
    inv_freq = (500000.0 ** (-np.arange(0, 16, 2, dtype=np.float32) / 16)).astype(np.float32)
    misc = np.zeros((128, 16), np.float32)
    for q in range(16):
        misc[q, 0] = inv_freq[q % 8]
        misc[q, 1] = -1.0 if q < 8 else 1.0
    sel = np.zeros((4, 128, 128), np.float32)
    for hh in range(2):
        for m in range(64):
            sel[hh, hh * 64 + m, m] = 1.0
        for m in range(16):
            sel[2 + hh, hh * 64 + (m + 8) % 16, m] = 1.0
    c["cst"] = np.concatenate([ident, tri, ustrict, iota32, ecap, rperm, misc, sel[0], sel[1], sel[2], sel[3]], axis=1).astype(np.float32)
    kaug = np.zeros((8, SEQ), np.float32)
    for n in range(8):
        kaug[n, n * 256:(n + 1) * 256] = 1.0
    c["kaug"] = kaug
    c["tokid"] = (np.arange(16, dtype=np.int32)[None, :] * 128 + p[:, None]).astype(np.int32)
    c["zero_i"] = np.zeros((128, NE * CAP // 128), np.int32)
    return c


CST_COLS = 128 * 3 + 32 + 32 + 16 + 16 + 512


def build(nc, NL, dbg=(), stop=None, NLW=4):
    S = Sched(nc)
    dbg_out = {}

    def din(name, shape, dt=F32):
        return nc.dram_tensor(name, shape, dt, kind="ExternalInput").ap()

    x_d = din("x", [SEQ, D])
    pos_d = din("pos", [1, SEQ], I32)
    w_in_d = din("w_in", [NLW, D, 2560])
    attn_gain_d = din("attn_gain", [NLW, 512])
    conv_w_d = din("conv_w", [NLW, 31, 512])
    conv_b_d = din("conv_b", [NLW, 512])
    conv_ln_g_d = din("conv_ln_g", [NLW, 512])
    conv_ln_b_d = din("conv_ln_b", [NLW, 512])
    w_o_d = din("w_o", [NLW, D, D])
    ln1_g_d = din("ln1_g", [NLW, D])
    ln1_b_d = din("ln1_b", [NLW, D])
    w_router_d = din("w_router", [NLW, D, NE])
    b_router_d = din("b_router", [NLW, NE])
    w_gu_d = din("w_gu", [NLW, NE, D, 2 * D])
    b_gu_d = din("b_gu", [NLW, NE, 2 * D])
    w_dn_d = din("w_dn", [NLW, NE, D, D])
    b_dn_d = din("b_dn", [NLW, NE, D])
    ln2_g_d = din("ln2_g", [NLW, D])
    ln2_b_d = din("ln2_b", [NLW, D])
    cst_d = din("cst", [128, CST_COLS])
    kaug_d = din("kaug", [8, SEQ])
    tokid_d = din("tokid", [128, 16], I32)
    zero_i_d = din("zero_i", [128, NE * CAP // 128], I32)
    out_d = nc.dram_tensor("out", [SEQ, D], F32, kind="ExternalOutput").ap()
    XTM_d = nc.dram_tensor("xtm_scr", [SEQ, D], BF16, kind="Internal").ap()
    S2T_d = nc.dram_tensor("s2t_scr", [NE * CAP, 1], I32, kind="Internal").ap()
    YS_d = nc.dram_tensor("ys_scr", [NE * CAP, D], F32, kind="Internal").ap()

    def sb(name, shape, dt):
        return nc.alloc_sbuf_tensor("sb_" + name, shape, dt)

    H = sb("H", [128, 8, SEQ], BF16)
    L = sb("L", [128, 8, SEQ], BF16)
    tH = S.toks("H", 4)
    tL = S.toks("L", 4)
    cst = sb("cst", [128, CST_COLS], F32)
    ident_f = cst[:, 0:128]
    tri_f = cst[:, 128:256]
    ustrict_f = cst[:, 256:384]
    iota32 = cst[:, 384:416]
    ecap = cst[:, 416:448]
    rperm_f = cst[:, 448:464]
    invf = cst[:, 464:465]
    ssign = cst[:, 465:466]
    cbf = sb("cbf", [128, 128 * 7 + 16 + 512], BF16)
    psel = [cbf[:, 912 + i * 128:912 + (i + 1) * 128] for i in range(4)]
    e0 = sb("e0", [128, 128], F32)
    br_pad = sb("br_pad", [128, NE], F32)
    ident_bf = cbf[:, 0:128]
    tri_bf = cbf[:, 128:256]
    ustrict_bf = cbf[:, 256:384]
    aI_hi = cbf[:, 384:512]
    aI_lo = cbf[:, 512:640]
    ones_bf = cbf[:, 640:768]
    onesD_bf = cbf[:, 768:896]
    rperm_bf = cbf[:, 896:912]
    onesC_bf = sb("onesC", [128, 128], BF16)
    tokid = sb("tokid", [128, 16], I32)
    tabC = sb("tabC", [128, SEQ], F32)
    tabS = sb("tabS", [128, SEQ], F32)
    tC = S.tok("cst")
    tCB = S.tok("cbf")
    tTab = S.tok("tab")
    tTokid = S.tok("tokid")
    pcol = sb("pcol", [128, 48], F32)
    convw = sb("convw", [128, 124], F32)
    bguT = sb("bguT", [128, 16, NE], F32)
    wr_hi = sb("wr_hi", [128, 8, NE], BF16)
    wr_lo = sb("wr_lo", [128, 8, NE], BF16)
    tP = S.tok("params")
    tBgu = S.tok("bgu")
    tWr = S.tok("wr")
    g4 = sb("g4", [128, 16, 4], F32)
    slot_i = sb("slot_i", [128, 16, 4], I32)
    mask_all = sb("mask_all", [128, 16, NE], BF16)
    idx_all = sb("idx_all", [128, NE, J], I32)
    tG4 = S.toks("g4", 16)
    tSlot = S.toks("slot", 16)
    tMask = S.toks("mask", 16)
    tIdx = S.tok("idx_all")

    ARENA = 114 * 1024
    arena = sb("arena", [128, ARENA // 2], BF16)

    def av(off, shape, dt):
        esz = 2 if dt == BF16 else 4
        n = 1
        for s in shape[1:]:
            n *= s
        assert off % 4 == 0 and off + n * esz <= ARENA, (off, shape)
        ap = arena[:, off // 2: off // 2 + n * esz // 2]
        if dt != BF16:
            ap = ap.bitcast(dt)
        if len(shape) == 3:
            ap = ap.rearrange("p (a b) -> p a b", a=shape[1])
        elif len(shape) == 4:
            ap = ap.rearrange("p (a b c) -> p a b c", a=shape[1], b=shape[2])
        return ap

    K = 1024
    mixT = av(0, [128, 8, SEQ], BF16)
    tMix = [[S.tok("mix%d_%d" % (f, tc)) for tc in range(4)] for f in range(8)]
    RING0 = 32 * K
    ring = [av(RING0 + i * 8 * K, [128, 8, 512], BF16) for i in range(6)]
    tRing = S.toks("ring", 6)
    PH0 = 80 * K
    phase_toks = []

    def new_phase(toks):
        nonlocal phase_toks
        S.alias(toks, phase_toks)
        phase_toks = list(toks)

    psum = [nc.alloc_psum_tensor("ps%d" % i, [128, 512], F32) for i in range(8)]
    tPs = S.toks("ps", 8)
    rot = {"big": [0, 0, 1, 2, 3], "acc": [0, 4, 5], "aux": [0, 6, 7]}

    def ps_next(group):
        r = rot[group]
        i = r[1 + r[0] % (len(r) - 1)]
        r[0] += 1
        return psum[i], tPs[i]

    def mm(out, lhsT, rhs, start, stop, reads, writes):
        S.op("pe", lambda e: e.matmul(out, lhsT=lhsT, rhs=rhs, start=start, stop=stop), reads, writes)

    def tr(out, in_, ident, reads, writes):
        S.op("pe", lambda e: e.transpose(out=out, in_=in_, identity=ident), reads, writes)

    def act(out, in_, func, reads, writes, **kw):
        S.op("act", lambda e: e.activation(out=out, in_=in_, func=func, **kw), reads, writes)

    def ts(eng, out, in0, s1, s2, op0, op1, reads, writes):
        if op1 is None:
            S.op(eng, lambda e: e.tensor_scalar(out=out, in0=in0, scalar1=s1, scalar2=None, op0=op0), reads, writes)
        else:
            S.op(eng, lambda e: e.tensor_scalar(out=out, in0=in0, scalar1=s1, scalar2=s2, op0=op0, op1=op1), reads, writes)

    def tt(eng, out, in0, in1, op, reads, writes):
        S.op(eng, lambda e: e.tensor_tensor(out=out, in0=in0, in1=in1, op=op), reads, writes)

    def stt(out, in0, scalar, in1, op0, op1, reads, writes):
        S.op("dve", lambda e: e.scalar_tensor_tensor(out=out, in0=in0, scalar=scalar, in1=in1, op0=op0, op1=op1), reads, writes)

    def cp(eng, out, in_, reads, writes):
        if eng == "act":
            S.op("act", lambda e: e.copy(out=out, in_=in_), reads, writes)
        else:
            S.op(eng, lambda e: e.tensor_copy(out=out, in_=in_), reads, writes)

    def dbg_dump(name, ap_sb, shape, reads):
        if name not in dbg:
            return
        d = nc.dram_tensor("dbg_" + name, shape, F32, kind="ExternalOutput").ap()
        t = S.tok("dbg_" + name)
        S.dma("pool", d, ap_sb, reads, t)
        dbg_out[name] = t

    def tcs(tc):
        return slice(tc * 512, (tc + 1) * 512)

    S.dma("sp", cst[:], cst_d, [], tC)
    S.dma("sp", tokid[:], tokid_d, [], tTokid)
    tS2T = S.toks("s2t", 16)
    for i in range(16):
        pass
    zi = av(PH0, [128, NE * CAP // 128], I32)
    tZi = S.tok("zi")
    S.dma("sp", zi, zero_i_d, [], tZi)
    tS2Tall = S.tok("s2t_init")
    S.dma("sp", S2T_d.rearrange("(p j) o -> p (j o)", p=128), zi, [tZi], tS2Tall)
    for t in tS2T:
        S.alias([t], [tS2Tall])
    cp("dve", cbf[:, 0:384], cst[:, 0:384], [tC], [tCB])
    hi_a = float(np.asarray(ALPHA, np.float32).astype(mybir_bf16()).astype(np.float32))
    lo_a = float(np.float32(ALPHA) - np.float32(hi_a))
    ts("dve", aI_hi, ident_f, hi_a, None, ALU.mult, None, [tC, tCB], [tCB])
    ts("dve", aI_lo, ident_f, lo_a, None, ALU.mult, None, [tC, tCB], [tCB])
    S.op("dve", lambda e: e.memset(ones_bf, 1.0), [tCB], [tCB])
    S.op("dve", lambda e: e.memset(onesD_bf, 1.0 / 1024), [tCB], [tCB])
    S.op("dve", lambda e: e.memset(onesC_bf[:], 1.0 / 512), [tCB], [tCB])
    cp("dve", rperm_bf, rperm_f, [tC, tCB], [tCB])
    cp("dve", cbf[:, 912:912 + 512], cst[:, 480:480 + 512], [tC, tCB], [tCB])
    S.op("dve", lambda e: e.memset(e0[:], 0.0), [tCB], [tCB])
    S.op("dve", lambda e: e.memset(e0[0:1, :], 1.0), [tCB], [tCB])
    S.op("dve", lambda e: e.memset(br_pad[:], 0.0), [tCB], [tWr])

    pos_i = av(0, [128, SEQ], I32)
    ang = av(8 * K, [128, SEQ], F32)
    kf = av(16 * K, [128, SEQ], F32)
    ki = av(24 * K, [128, SEQ], I32)
    tA = S.tok("ang")
    tPi = S.tok("pos_i")
    S.dma("sp", pos_i[0:16, :], pos_d.to_broadcast([16, SEQ]), [], tPi)
    P16 = slice(0, 16)
    cp("dve", ang[P16], pos_i[P16], [tPi], [tA])
    ts("dve", ang[P16], ang[P16], invf[P16], None, ALU.mult, None, [tA, tC], [tA])
    ts("dve", kf[P16], ang[P16], float(1.0 / (2 * np.pi)), None, ALU.mult, None, [tA], [tA])
    cp("dve", ki[P16], kf[P16], [tA], [tA])
    cp("dve", kf[P16], ki[P16], [tA], [tA])
    c1 = float(np.float32(2 * np.pi))
    c2 = float(2 * np.pi - np.float64(np.float32(2 * np.pi)))
    stt(ang[P16], kf[P16], -c1, ang[P16], ALU.mult, ALU.add, [tA], [tA])
    stt(ang[P16], kf[P16], -c2, ang[P16], ALU.mult, ALU.add, [tA], [tA])
    S.op("dve", lambda e: e.memset(tabC[:], 1.0), [], [tTab])
    S.op("dve", lambda e: e.memset(tabS[:], 0.0), [], [tTab])
    for (tab, shift) in ((tabS, 0.0), (tabC, float(np.pi / 2))):
        y = kf
        ts("dve", y[P16], ang[P16], shift, None, ALU.add, None, [tA], [tA])
        corr = ki.bitcast(F32)
        ts("dve", corr[P16], y[P16], float(np.pi), float(-2 * np.pi), ALU.is_gt, ALU.mult, [tA], [tA])
        tt("dve", y[P16], y[P16], corr[P16], ALU.add, [tA], [tA])
        ts("dve", corr[P16], y[P16], float(-np.pi), float(2 * np.pi), ALU.is_lt, ALU.mult, [tA], [tA])
        tt("dve", y[P16], y[P16], corr[P16], ALU.add, [tA], [tA])
        ts("dve", y[P16], y[P16], PI, -PI, ALU.min, ALU.max, [tA], [tA])
        if shift == 0.0:
            act(tab[P16], y[P16], AF.Sin, [tA, tC], [tTab], scale=ssign[P16])
        else:
            act(tab[P16], y[P16], AF.Sin, [tA], [tTab])

    xin = [av(RING0 + i * 4 * K, [128, D], F32) for i in range(2)]
    tXin = S.toks("xin", 2)
    init_toks = [tZi, tA, tPi] + tXin

    def split_hilo(src_f32, c, cols, tcw, reads):
        cp("act", H[:, c, cols], src_f32, reads, [tH[tcw]])
        tt("dve", L[:, c, cols], src_f32, H[:, c, cols], ALU.subtract, reads + [tH[tcw]], [tL[tcw]])

    for tti in range(16):
        xi = xin[tti % 2]
        S.dma("sp", xi, x_d[tti * 128:(tti + 1) * 128, :], [], tXin[tti % 2])
        for half in range(2):
            ps, tps = ps_next("aux")
            for cc in range(4):
                c = half * 4 + cc
                tr(ps[:, cc * 128:(cc + 1) * 128], xi[:, c * 128:(c + 1) * 128], ident_f, [tXin[tti % 2], tC], [tps])
            for cc in range(4):
                c = half * 4 + cc
                split_hilo(ps[:, cc * 128:(cc + 1) * 128], c, slice(tti * 128, (tti + 1) * 128), tti // 4, [tps])
    phase_toks = init_toks
    S.alias(tRing, tXin)

    def ln_fm(z, zb, sd, rstd, tZ, gcol, bcol, tc, n_ch, ones_ap, dst_hl, dst_mix=None, silu=False, tdst=None):
        cp("pool", zb, z, [tZ], [tZ])
        pm, tpm = ps_next("acc")
        for c in range(n_ch):
            mm(pm[:], ones_ap, zb[:, c, :], c == 0, c == n_ch - 1, [tZ, tCB], [tpm])
        for c in range(n_ch):
            tt("dve", z[:, c, :], z[:, c, :], pm[:], ALU.subtract, [tZ, tpm], [tZ])
        act(zb, z, AF.Square, [tZ], [tZ])
        pv, tpv = ps_next("acc")
        for c in range(n_ch):
            mm(pv[:], ones_ap, zb[:, c, :], c == 0, c == n_ch - 1, [tZ, tCB], [tpv])
        act(sd, pv[:], AF.Sqrt, [tpv, tCB], [tZ], bias=epsc[:, 0:1])
        S.op("dve", lambda e: e.reciprocal(out=rstd, in_=sd), [tZ], [tZ])
        for c in range(n_ch):
            tt("dve", z[:, c, :], z[:, c, :], rstd, ALU.mult, [tZ], [tZ])
        for c in range(n_ch):
            if silu:
                act(dst_mix[:, c, :], z[:, c, :], AF.Silu, [tZ, tP], [tdst[c]], scale=pcol[:, gcol + c:gcol + c + 1],
                    bias=pcol[:, bcol + c:bcol + c + 1])
            else:
                ts("pool", z[:, c, :], z[:, c, :], pcol[:, gcol + c:gcol + c + 1], pcol[:, bcol + c:bcol + c + 1], ALU.mult, ALU.add,
                   [tZ, tP], [tZ])
                split_hilo(z[:, c, :], c, tcs(tc), tc, [tZ])

    epsc = sb("epsc", [128, 1], F32)
    S.op("dve", lambda e: e.memset(epsc[:], EPS), [], [tCB])

    order = ['init', 'pre', 'conv', 'att', 'p3', 'moe', 'cmb']
    lim = order.index(stop) if stop else 99
    for l in range(NL if lim > 0 else 0):
        st1 = av(PH0, [128, 128], F32)
        st2 = av(PH0 + 512, [128, 128], F32)
        st3 = av(PH0 + 1 * K, [128, 2 * D], F32)
        wr_f = av(PH0 + 9 * K, [128, 8, NE], F32)
        tSt = S.toks("st", 4)
        S.alias([t for f in range(8) for t in tMix[f]], phase_toks)
        new_phase(tSt)
        vecs = [(ln1_g_d, 8), (ln1_b_d, 8), (ln2_g_d, 8), (ln2_b_d, 8), (conv_b_d, 4), (conv_ln_g_d, 4), (conv_ln_b_d, 4),
                (attn_gain_d, 4)]
        S.op("pool", lambda e: e.memset(st1, 0.0), [], [tSt[0]])
        S.op("pool", lambda e: e.memset(st2, 0.0), [], [tSt[1]])
        S.op("pool", lambda e: e.memset(st3, 0.0), [], [tSt[2]])
        r0 = 0
        for (vd, n) in vecs:
            S.dma("sp", st1[r0:r0 + n, :], vd[l].rearrange("(n p) -> n p", p=128), [], tSt[0])
            r0 += n
        S.dma("sp", st2[0:124, :], conv_w_d[l].rearrange("j (c p) -> (j c) p", p=128), [], tSt[1])
        S.dma("sp", st3[0:NE, :], b_gu_d[l], [], tSt[2])
        S.dma("sp", wr_f, w_router_d[l].rearrange("(c p) e -> p c e", p=128), [], tSt[3])
        S.dma("sp", br_pad[0:1, :], b_router_d[l:l + 1, :], [], tWr)
        ps, tps = ps_next("aux")
        tr(ps[:, 0:128], st1[:, :], ident_f, [tSt[0], tC], [tps])
        cp("dve", pcol[:], ps[:, 0:48], [tps], [tP])
        ps, tps = ps_next("aux")
        tr(ps[:, 0:128], st2[:, :], ident_f, [tSt[1], tC], [tps])
        cp("dve", convw[:], ps[:, 0:124], [tps], [tP])
        ps, tps = ps_next("aux")
        for g in range(4):
            ps, tps = ps_next("aux")
            for c4 in range(4):
                c = g * 4 + c4
                tr(ps[:, c4 * 128:(c4 + 1) * 128], st3[:, c * 128:(c + 1) * 128], ident_f, [tSt[2], tC], [tps])
            cp("dve", bguT[:, g * 4:(g + 1) * 4, :], ps[:].rearrange("p (a b) -> p a b", a=4)[:, :, 0:NE], [tps], [tBgu])
        ts("dve", bguT[:, 8:16, :], bguT[:, 8:16, :], 1.0, None, ALU.add, None, [tBgu], [tBgu])
        cp("dve", wr_hi[:], wr_f, [tSt[3]], [tWr])
        tt("dve", wr_lo[:], wr_f, wr_hi[:], ALU.subtract, [tSt[3], tWr], [tWr])

        if lim < 2:
            break
        u = av(PH0, [128, SEQ + 32], BF16)
        diag = av(PH0 + 5 * K, [128, 31, 128], BF16)
        sg = av(PH0 + 13 * K, [128, 512], F32)
        zc = av(PH0 + 15 * K, [128, 4, 512], F32)
        zcb = av(PH0 + 23 * K, [128, 4, 512], BF16)
        sdc = av(PH0 + 27 * K, [128, 512], F32)
        rsc = av(PH0 + 29 * K, [128, 512], F32)
        tU = S.tok("u")
        tDg = S.tok("diag")
        tSg = S.tok("sg")
        tZc = S.tok("zc")
        new_phase([tU, tDg, tSg, tZc])
        for cc in range(4):
            sl = cc % 2
            wsl = ring[sl]
            S.dma("pool", wsl[:, :, 0:128], w_in_d[l, :, 1536 + cc * 128:1536 + (cc + 1) * 128].rearrange("(c p) n -> p c n", p=128),
                  [], tRing[sl])
            S.dma("pool", wsl[:, :, 128:256], w_in_d[l, :, 2048 + cc * 128:2048 + (cc + 1) * 128].rearrange("(c p) n -> p c n", p=128),
                  [], tRing[sl])
            S.op("dve", lambda e: e.memset(u[:, 0:32], 0.0), [], [tU])
            for j in range(31):
                col = j * 4 + cc
                ts("dve", diag[:, j, :], ident_bf, convw[:, col:col + 1], None, ALU.mult, None, [tCB, tP], [tDg])
            for tc in range(4):
                pa, tpa = ps_next("big")
                pg, tpg = ps_next("big")
                for c in range(8):
                    mm(pa[:], wsl[:, c, 0:128], H[:, c, tcs(tc)], c == 0, c == 7, [tRing[sl], tH[tc]], [tpa])
                for c in range(8):
                    mm(pg[:], wsl[:, c, 128:256], H[:, c, tcs(tc)], c == 0, c == 7, [tRing[sl], tH[tc]], [tpg])
                act(sg, pg[:], AF.Sigmoid, [tpg], [tSg])
                tt("dve", u[:, 32 + tc * 512:32 + (tc + 1) * 512], pa[:], sg, ALU.mult, [tpa, tSg], [tU])
            for tc in range(4):
                pc, tpc = ps_next("big")
                for j in range(31):
                    o0 = 32 + tc * 512 - 30 + j
                    mm(pc[:], diag[:, j, :], u[:, o0:o0 + 512], j == 0, j == 30, [tDg, tU], [tpc])
                act(mixT[:, 4 + cc, tcs(tc)], pc[:], AF.Identity, [tpc, tP], [tMix[4 + cc][tc]], bias=pcol[:, 32 + cc:33 + cc])
        for tc in range(4):
            for cc in range(4):
                cp("dve", zc[:, cc, :], mixT[:, 4 + cc, tcs(tc)], [tMix[4 + cc][tc]], [tZc])
            ln_fm(zc, zcb, sdc, rsc, tZc, 36, 40, tc, 4, onesC_bf[:], None, dst_mix=mixT[:, 4:8, tcs(tc)], silu=True,
                  tdst=[tMix[4 + cc][tc] for cc in range(4)])
        dbg_dump("yconv", mixT[:, 4:8, :], [128, 4, SEQ], [tMix[4 + cc][tc] for cc in range(4) for tc in range(4)])

        if lim < 3:
            break
        qT = av(PH0, [128, SEQ], BF16)
        kT = av(PH0 + 4 * K, [128, SEQ], BF16)
        Vp = av(PH0 + 8 * K, [128, 16, 128], BF16)
        PTs = [av(PH0 + 12 * K + i * K, [128, 512], BF16) for i in range(4)]
        t1 = av(PH0 + 16 * K, [128, 512], F32)
        t2 = av(PH0 + 18 * K, [128, 512], F32)
        G = av(PH0 + 20 * K, [128, 8, 8], F32)
        mx = av(PH0 + 20 * K + 256, [128, 8, 8], F32)
        km = av(PH0 + 20 * K + 512, [128, 8], F32)
        km_bf = av(PH0 + 20 * K + 512 + 32, [128, 8], BF16)
        Bp = av(PH0 + 21 * K, [128, 8, 128], BF16)
        rden = av(PH0 + 23 * K, [128, 512], F32)
        sqa = av(PH0 + 25 * K, [128, 4, 512], BF16)
        sda = av(PH0 + 29 * K, [128, 512], F32)
        rsa = av(PH0 + 31 * K, [128, 512], F32)
        tQ = S.tok("qT")
        tK = S.tok("kT")
        tQa = S.tok("qTaug")
        tV = S.tok("Vp")
        tPT = S.toks("PT", 4)
        tT1 = S.tok("t1")
        tT2 = S.tok("t2")
        tG = S.tok("G")
        tKm = S.tok("km")
        tBp = S.tok("Bp")
        tRd = S.tok("rden")
        tSq = S.tok("sqa")
        new_phase([tQ, tK, tQa, tV, tT1, tT2, tG, tKm, tBp, tRd, tSq] + tPT)
        S.op("pool", lambda e: e.memset(kT[64:128, :], 0.0), [], [tK])
        S.dma("pool", kT[64:72, :], kaug_d, [], tK)
        S.op("pool", lambda e: e.memset(qT[64:128, :], 0.0), [], [tQa])
        S.op("pool", lambda e: e.memset(km_bf, 0.0), [], [tKm])
        qpair = av(PH0 + 33 * K, [128, 512], BF16)
        wqk_f = av(RING0 + 4 * 8 * K, [128, 8, 256], F32)
        wlo = av(RING0 + 5 * 8 * K, [128, 8, 256], BF16)
        qpair_lo = av(RING0 + 5 * 8 * K + 4 * K, [128, 512], BF16)
        qlo = av(RING0 + 5 * 8 * K + 5 * K, [128, 1024], BF16)
        km_lo = av(RING0 + 5 * 8 * K + 7 * K, [128, 8], BF16)
        tQlo = S.tok("qlo")
        S.alias([tQlo], [tRing[5]])
        S.op("pool", lambda e: e.memset(qlo, 0.0), [], [tQlo])
        S.op("pool", lambda e: e.memset(km_lo, 0.0), [tKm], [tKm])
        tQp = S.tok("qpair")
        S.alias([tQp], [tSq])
        S.op("pool", lambda e: e.memset(Bp, 0.0), [], [tBp])
        PTn = [0]
        AL = int(os.environ.get('ATT_LIM', '9'))
        for jp in range(4 if AL > 1 else 0):
            sl = jp % 2
            wsl = ring[sl]
            for part in range(3):
                S.dma("pool", wsl[:, :, part * 128:(part + 1) * 128],
                      w_in_d[l, :, part * 512 + jp * 128:part * 512 + (jp + 1) * 128].rearrange("(c p) n -> p c n", p=128),
                      [], tRing[sl])
            for part in range(2):
                S.dma("sp", wqk_f[:, :, part * 128:(part + 1) * 128],
                      w_in_d[l, :, part * 512 + jp * 128:part * 512 + (jp + 1) * 128].rearrange("(c p) n -> p c n", p=128),
                      [], tRing[4])
            tt("dve", wlo, wqk_f, wsl[:, :, 0:256], ALU.subtract, [tRing[4], tRing[sl]], [tRing[5]])
            for g4i in range(4):
                ps, tps = ps_next("big")
                for kk in range(4):
                    kt = g4i * 4 + kk
                    for c in range(8):
                        mm(ps[:, kk * 128:(kk + 1) * 128], H[:, c, kt * 128:(kt + 1) * 128], wsl[:, c, 256:384], c == 0, c == 7,
                           [tH[kt // 4], tRing[sl]], [tps])
                cp("act", Vp[:, g4i * 4:(g4i + 1) * 4, :].rearrange("p a b -> p (a b)"), ps[:], [tps], [tV])
            for hh in range(2 if AL > 2 else 0):
                h = 2 * jp + hh
                for (dst, tD, col0) in ((kT, tK, 128), (qT, tQ, 0)):
                    for tc in range(4):
                        ps, tps = ps_next("big")
                        for c in range(8):
                            mm(ps[:], wsl[:, c, col0:col0 + 128], H[:, c, tcs(tc)], c == 0, False, [tRing[sl], tH[tc]], [tps])
                            mm(ps[:], wsl[:, c, col0:col0 + 128], L[:, c, tcs(tc)], False, False, [tRing[sl], tL[tc]], [tps])
                            mm(ps[:], wlo[:, c, col0:col0 + 128], H[:, c, tcs(tc)], False, c == 7, [tRing[5], tH[tc]], [tps])
                        cp("act", qpair, ps[:], [tps], [tQp])
                        tt("dve", qpair_lo, ps[:], qpair, ALU.subtract, [tps, tQp], [tQp])
                        pa_, tpa_ = ps_next("big")
                        mm(pa_[:], psel[hh], qpair, True, False, [tQp, tCB], [tpa_])
                        mm(pa_[:], psel[hh], qpair_lo, False, True, [tQp, tCB], [tpa_])
                        pr, tpr = ps_next("aux")
                        mm(pr[:], psel[2 + hh], qpair, True, False, [tQp, tCB], [tpr])
                        mm(pr[:], psel[2 + hh], qpair_lo, False, True, [tQp, tCB], [tpr])
                        tt("dve", t1[0:64, :], pa_[0:64, :], tabC[0:64, tcs(tc)], ALU.mult, [tpa_, tTab], [tT1])
                        tt("dve", t2[0:64, :], pr[0:64, :], tabS[0:64, tcs(tc)], ALU.mult, [tpr, tTab], [tT2])
                        tt("dve", t1[0:64, :], t1[0:64, :], t2[0:64, :], ALU.add, [tT1, tT2], [tT1])
                        cp("act", dst[0:64, tcs(tc)], t1[0:64, :], [tT1], [tD])
                        if dst is qT and tc >= 2:
                            tt("dve", qlo[0:64, (tc - 2) * 512:(tc - 1) * 512], t1[0:64, :], dst[0:64, tcs(tc)], ALU.subtract,
                               [tT1, tD], [tQlo])
                        if dst is kT:
                            S.op("dve", lambda e, tc=tc: e.tensor_reduce(out=km[0:64, tc * 2:tc * 2 + 2],
                                                                         in_=t1[0:64, :].rearrange("p (n k) -> p n k", k=256),
                                                                         axis=AX.X, op=ALU.add), [tT1], [tKm])
                if "q" in dbg and l == 0 and h == 1:
                    dbg_dump("q", qT[0:64, :], [64, SEQ], [tQ])
                    dbg_dump("k", kT[0:64, :], [64, SEQ], [tK])
                if AL < 4:
                    continue
                ts("dve", km[0:64, :], km[0:64, :], 1.0 / 256, None, ALU.mult, None, [tKm], [tKm])
                cp("dve", km_bf[0:64, :], km[0:64, :], [tKm], [tKm])
                tt("dve", km_lo[0:64, :], km[0:64, :], km_bf[0:64, :], ALU.subtract, [tKm], [tKm])
                pg, tpg = ps_next("aux")
                for qi in range(8):
                    qt = 8 + qi
                    mm(pg[:, qi * 8:(qi + 1) * 8], qT[:, qt * 128:(qt + 1) * 128], km_bf[:, :], True, False, [tQ, tQa, tKm], [tpg])
                    mm(pg[:, qi * 8:(qi + 1) * 8], qlo[:, qi * 128:(qi + 1) * 128], km_bf[:, :], False, False, [tQlo, tKm], [tpg])
                    mm(pg[:, qi * 8:(qi + 1) * 8], qT[:, qt * 128:(qt + 1) * 128], km_lo[:, :], False, True, [tQ, tQa, tKm], [tpg])
                cp("dve", G.rearrange("p a b -> p (a b)"), pg[:, 0:64], [tpg], [tG])
                for own in range(4, 8):
                    qi0 = (own - 4) * 2
                    S.op("dve", lambda e, qi0=qi0, own=own: e.memset(G[:, qi0:qi0 + 2, own:8], -1e30), [tG], [tG])
                for qi in range(8):
                    own = (8 + qi) // 2
                    S.op("dve", lambda e, qi=qi: e.max(out=mx[:, qi, :], in_=G[:, qi, :]), [tG], [tG])
                    ts("dve", Bp[:, qi, 64:64 + own], G[:, qi, 0:own], mx[:, qi, 2:3], NEG, ALU.is_lt, ALU.mult, [tG], [tBp])
                if AL < 5:
                    continue
                for half in range(2):
                    pb, tpb = ps_next("aux")
                    for i in range(4):
                        qi = half * 4 + i
                        mm(pb[:, i * 128:(i + 1) * 128], Bp[:, qi, :], ident_bf, True, True, [tBp, tCB], [tpb])
                    cp("dve", qT[64:128, 1024 + half * 512:1024 + (half + 1) * 512], pb[64:128, :], [tpb], [tQa])
                if AL < 6:
                    continue
                rows = slice(hh * 64, (hh + 1) * 64)
                for c in range(4):
                    nkt = 4 * c + 4
                    po, tpo = psum[4], tPs[4]
                    pd, tpd = psum[5], tPs[5]
                    for kt in range(nkt):
                        q0 = max(kt * 128, c * 512)
                        off = q0 - c * 512
                        pss, tpss = ps_next("big")
                        diagt = kt >= 4 * c
                        mm(pss[:, off:512], kT[:, kt * 128:(kt + 1) * 128], qT[:, q0:(c + 1) * 512], True, not diagt,
                           [tK, tQ, tQa], [tpss])
                        if diagt:
                            mm(pss[:, off:off + 128], ident_bf, tri_bf, False, True, [tCB], [tpss])
                        pi = PTn[0] % 4
                        PTn[0] += 1
                        act(PTs[pi][:, off:512], pss[:, off:512], AF.Exp, [tpss], [tPT[pi]], scale=0.125)
                        mm(po[:, off:512], Vp[:, kt, :], PTs[pi][:, off:512], kt == 0, kt == nkt - 1,
                           [tV, tPT[pi]], [tpo])
                        mm(pd[:, off:512], ones_bf, PTs[pi][:, off:512], kt == 0, kt == nkt - 1, [tCB, tPT[pi]], [tpd])
                    S.op("dve", lambda e, rows=rows, pd=pd: e.reciprocal(out=rden[rows, :], in_=pd[rows, :]), [tpd], [tRd])
                    tt("dve", mixT[rows, jp, tcs(c)], po[rows, :], rden[rows, :], ALU.mult, [tpo, tRd], [tMix[jp][c]])
        dbg_dump("att", mixT[:, 0:4, :], [128, 4, SEQ], [tMix[f][tc] for f in range(4) for tc in range(4)])
        for tc in range(4 if AL >= 6 else 0):
            rd4 = [tMix[f][tc] for f in range(4)]
            act(sqa, mixT[:, 0:4, tcs(tc)], AF.Square, rd4, [tSq])
            pv, tpv = ps_next("acc")
            for f in range(4):
                mm(pv[:], onesC_bf[:], sqa[:, f, :], f == 0, f == 3, [tSq, tCB], [tpv])
            act(sda, pv[:], AF.Sqrt, [tpv], [tSq], bias=epsc[:, 0:1])
            S.op("dve", lambda e: e.reciprocal(out=rsa, in_=sda), [tSq], [tSq])
            for f in range(4):
                stt(mixT[:, f, tcs(tc)], mixT[:, f, tcs(tc)], pcol[:, 44 + f:45 + f], rsa, ALU.mult, ALU.mult, [tSq, tP, tMix[f][tc]],
                    [tMix[f][tc]])
        dbg_dump("yattn", mixT[:, 0:4, :], [128, 4, SEQ], [tMix[f][tc] for f in range(4) for tc in range(4)])

        if lim < 4:
            break
        z = av(PH0, [128, 8, 512], F32)
        zb = av(PH0 + 16 * K, [128, 8, 512], BF16)
        sd = av(PH0 + 24 * K, [128, 512], F32)
        rstd = av(PH0 + 26 * K, [128, 512], F32)
        tZ = S.tok("z")
        new_phase([tZ])
        for half in range(2):
            S.dma("pool", ring[2 + half], w_o_d[l, :, half * 512:(half + 1) * 512].rearrange("(c p) n -> p c n", p=128), [], tRing[2 + half])
        for tc in range(4):
            for cch in range(8):
                ps, tps = ps_next("big")
                wsl = ring[2 + cch // 4]
                tw = tRing[2 + cch // 4]
                cs = slice((cch % 4) * 128, (cch % 4 + 1) * 128)
                for f in range(8):
                    mm(ps[:], wsl[:, f, cs], mixT[:, f, tcs(tc)], f == 0, False, [tw, tMix[f][tc]], [tps])
                mm(ps[:], aI_hi, H[:, cch, tcs(tc)], False, False, [tCB, tH[tc]], [tps])
                mm(ps[:], aI_hi, L[:, cch, tcs(tc)], False, False, [tCB, tL[tc]], [tps])
                mm(ps[:], aI_lo, H[:, cch, tcs(tc)], False, True, [tCB, tH[tc]], [tps])
                cp("act", z[:, cch, :], ps[:], [tps], [tZ])
            ln_fm(z, zb, sd, rstd, tZ, 0, 8, tc, 8, onesD_bf, True)
        if "x1" in dbg and l == 0:
            dbg_dump("x1h", H[:], [128, 8, SEQ], tH)
            dbg_dump("x1l", L[:], [128, 8, SEQ], tL)

        if lim < 5:
            break
        xtm = [av(0 + i * 2 * K, [128, D], BF16) for i in range(2)]
        lg = av(4 * K, [128, NE], F32)
        mx8 = av(4 * K + 128, [128, 8], F32)
        idx8 = av(4 * K + 160, [128, 8], U32)
        nm = av(4 * K + 192, [128, 1], F32)
        e4 = av(4 * K + 196, [128, 4], F32)
        se = av(4 * K + 212, [128, 1], F32)
        rs = av(4 * K + 216, [128, 1], F32)
        idxf = av(4 * K + 220, [128, 4], F32)
        slotall = av(4 * K + 256, [128, NE], F32)
        oh = av(4 * K + 384, [128, NE], F32)
        slotf = av(4 * K + 512, [128, 4], F32)
        xg = [av(5 * K + i * 8 * K, [128, JS, D], BF16) for i in range(2)]
        xgT = av(21 * K, [128, 8, CAPS], BF16)
        hT = av(PH0 + 26 * K, [128, 8, CAPS], BF16)
        gcl = av(PH0, [128, CAPS], F32)
        gs = av(PH0 + 2 * K, [128, CAPS], F32)
        Al = av(PH0 + 4 * K, [128, CAPS], F32)
        y_sb = av(PH0 + 6 * K, [128, JS, D], F32)
        bdn = [av(PH0 + 22 * K, [128, D], F32) for i in range(2)]
        tXtm = S.toks("xtm", 2)
        tXTMd = S.toks("xtmd", 2)
        tR = S.tok("router")
        tXg = S.toks("xg", 2)
        tXgT = S.tok("xgT")
        tHT = S.tok("hT")
        tAct = S.toks("actscr", 3)
        tYsb = S.tok("ysb")
        tBdn = [S.tok("bdn0"), S.tok("bdn0")]
        tYS = S.toks("YS", 8)
        moe_toks = tXtm + [tR] + tXg + [tXgT, tHT] + tAct + [tYsb] + tBdn
        S.alias(moe_toks, phase_toks + [t for f in range(8) for t in tMix[f]])
        phase_toks = list(moe_toks)
        for tti in range(16):
            tc = tti // 4
            tsl = slice(tti * 128, (tti + 1) * 128)
            ps, tps = ps_next("aux")
            psb = ps[:].bitcast(BF16)
            for c in range(8):
                tr(psb[:, c * 128:(c + 1) * 128], H[:, c, tsl], ident_bf, [tH[tc], tCB], [tps])
            xi = tti % 2
            cp("act", xtm[xi], psb, [tps], [tXtm[xi]])
            S.dma("sp", XTM_d[tsl, :], xtm[xi], [tXtm[xi]], tXTMd[xi])
            pl, tpl = ps_next("aux")
            first = True
            for c in range(8):
                for (a, b, ta) in ((H, wr_hi, tH[tc]), (L, wr_hi, tL[tc]), (H, wr_lo, tH[tc])):
                    mm(pl[:, 0:NE], a[:, c, tsl], b[:, c, :], first, False, [ta, tWr], [tpl])
                    first = False
            mm(pl[:, 0:NE], e0[:], br_pad[:], False, True, [tCB, tWr], [tpl])
            cp("dve", lg, pl[:, 0:NE], [tpl], [tR])
            S.op("dve", lambda e: e.max(out=mx8, in_=lg), [tR], [tR])
            S.op("dve", lambda e: e.max_index(out=idx8, in_max=mx8, in_values=lg), [tR], [tR])
            ts("dve", mask_all[:, tti, :], lg, mx8[:, 3:4], None, ALU.is_ge, None, [tR], [tMask[tti]])
            ts("dve", nm, mx8[:, 0:1], -1.0, None, ALU.mult, None, [tR], [tR])
            act(e4, mx8[:, 0:4], AF.Exp, [tR], [tR], bias=nm, accum_out=se)
            S.op("dve", lambda e: e.reciprocal(out=rs, in_=se), [tR], [tR])
            ts("dve", g4[:, tti, :], e4, rs, None, ALU.mult, None, [tR], [tG4[tti]])
            cp("dve", idxf, idx8[:, 0:4], [tR], [tR])
            pr, tpr = ps_next("aux")
            for t2i in range(tti):
                mm(pr[:, 0:NE], ones_bf, mask_all[:, t2i, :], t2i == 0, False, [tCB, tMask[t2i]], [tpr])
            mm(pr[:, 0:NE], ustrict_bf, mask_all[:, tti, :], tti == 0, True, [tCB, tMask[tti]], [tpr])
            tt("dve", slotall, pr[:, 0:NE], ecap, ALU.add, [tpr, tC], [tR])
            for k in range(4):
                ts("dve", oh, iota32, idxf[:, k:k + 1], None, ALU.is_equal, None, [tR, tC], [tR])
                tt("dve", oh, oh, slotall, ALU.mult, [tR], [tR])
                S.op("dve", lambda e, k=k: e.reduce_sum(out=slotf[:, k:k + 1], in_=oh, axis=AX.X), [tR], [tR])
            cp("dve", slot_i[:, tti, :], slotf, [tR], [tSlot[tti]])
            for k in range(4):
                S.op("pool", lambda e, tti=tti, k=k: e.indirect_dma_start(
                    out=S2T_d, out_offset=bass.IndirectOffsetOnAxis(ap=slot_i[:, tti, k:k + 1], axis=0),
                    in_=tokid[:, tti:tti + 1], in_offset=None), [tSlot[tti], tTokid], [tS2T[tti]], dma=True)
        S.dma("sp", idx_all[:], S2T_d.rearrange("(e p j) o -> p e (j o)", p=128, j=J), tS2T, tIdx)
        if "slot" in dbg and l == 0:
            dbg_dump("slot", slot_i[:].rearrange("p a b -> p (a b)"), [128, 64], tSlot)
            dbg_dump("g4", g4[:].rearrange("p a b -> p (a b)"), [128, 64], tG4)

        def load_w(e, s):
            if e >= NE:
                return
            if s < 4:
                src = w_gu_d[l, e, :, s * 512:(s + 1) * 512]
            else:
                src = w_dn_d[l, e, :, (s - 4) * 512:(s - 3) * 512]
            S.dma("pool", ring[s], src.rearrange("(c p) n -> p c n", p=128), [], tRing[s])

        def gather(un):
            e, sbi = un // NSB, un % NSB
            if e >= NE:
                return
            for j in range(JS):
                S.op("pool", lambda en, e=e, j=j, un=un, sbi=sbi: en.indirect_dma_start(
                    out=xg[un % 2][:, j, :], out_offset=None, in_=XTM_d,
                    in_offset=bass.IndirectOffsetOnAxis(ap=idx_all[:, e, sbi * JS + j:sbi * JS + j + 1], axis=0)),
                    [tIdx] + tXTMd, [tXg[un % 2]], dma=True)
            if sbi == 0:
                S.dma("sp", bdn[0], b_dn_d[l, e:e + 1, :].to_broadcast([128, D]), [], tBdn[0])

        gather(0)
        for s_ in range(6):
            load_w(0, s_)
        for e in range(NE):
            for sbi in range(NSB):
                un = e * NSB + sbi
                last = sbi == NSB - 1
                gather(un + 1)
                for j in range(JS):
                    ps, tps = ps_next("aux")
                    psb = ps[:].bitcast(BF16)
                    for c in range(8):
                        tr(psb[:, c * 128:(c + 1) * 128], xg[un % 2][:, j, c * 128:(c + 1) * 128], ident_bf, [tXg[un % 2], tCB], [tps])
                    cp("act", xgT[:, :, j * 128:(j + 1) * 128], psb.rearrange("p (c t) -> p c t", c=8), [tps], [tXgT])
                for fch in range(8):
                    pg, tpg = ps_next("big")
                    pl, tpl = ps_next("big")
                    sg_, sl_ = fch // 4, 2 + fch // 4
                    fc = slice((fch % 4) * 128, (fch % 4 + 1) * 128)
                    for c in range(8):
                        mm(pg[:, 0:CAPS], ring[sg_][:, c, fc], xgT[:, c, :], c == 0, c == 7, [tRing[sg_], tXgT], [tpg])
                    for c in range(8):
                        mm(pl[:, 0:CAPS], ring[sl_][:, c, fc], xgT[:, c, :], c == 0, c == 7, [tRing[sl_], tXgT], [tpl])
                    ts("dve", gcl, pg[:, 0:CAPS], bguT[:, fch, e:e + 1], 7.0, ALU.add, ALU.min, [tpg, tBgu], [tAct[0]])
                    act(gs, gcl, AF.Gelu_apprx_sigmoid, [tAct[0]], [tAct[1]])
                    ts("dve", Al, pl[:, 0:CAPS], bguT[:, 8 + fch, e:e + 1], 8.0, ALU.add, ALU.min, [tpl, tBgu], [tAct[2]])
                    stt(hT[:, fch, :], Al, -6.0, gs, ALU.max, ALU.mult, [tAct[2], tAct[1]], [tHT])
                    if last and fch == 3:
                        load_w(e + 1, 0)
                        load_w(e + 1, 2)
                    if last and fch == 7:
                        load_w(e + 1, 1)
                        load_w(e + 1, 3)
                for half in range(2):
                    for j in range(JS):
                        ps, tps = ps_next("big")
                        for f in range(8):
                            mm(ps[:], hT[:, f, j * 128:(j + 1) * 128], ring[4 + half][:, f, :], f == 0, f == 7, [tHT, tRing[4 + half]], [tps])
                        tt("dve", y_sb[:, j, half * 512:(half + 1) * 512], ps[:], bdn[0][:, half * 512:(half + 1) * 512], ALU.add,
                           [tps, tBdn[0]], [tYsb])
                    if last:
                        load_w(e + 1, 4 + half)
                S.dma("sp", YS_d[e * CAP:(e + 1) * CAP, :].rearrange("(p j) d -> p j d", j=J)[:, sbi * JS:(sbi + 1) * JS, :], y_sb, [tYsb],
                      tYS[un % 8])

        Yk = [av(i * 4 * K, [128, D], F32) for i in range(4)]
        accs = av(16 * K, [128, D], F32)
        z2 = av(PH0, [128, 8, 512], F32)
        zb2 = av(PH0 + 16 * K, [128, 8, 512], BF16)
        sd2 = av(PH0 + 24 * K, [128, 512], F32)
        rstd2 = av(PH0 + 26 * K, [128, 512], F32)
        tYk = S.toks("Yk", 4)
        tAcc = S.tok("acc")
        tZ2 = S.tok("z2")
        new_phase(tYk + [tAcc, tZ2])
        for tti in range(16):
            tc = tti // 4
            tsl = slice(tti * 128, (tti + 1) * 128)
            for k in range(4):
                S.op("pool", lambda en, tti=tti, k=k: en.indirect_dma_start(
                    out=Yk[k], out_offset=None, in_=YS_d,
                    in_offset=bass.IndirectOffsetOnAxis(ap=slot_i[:, tti, k:k + 1], axis=0)),
                    [tSlot[tti]] + tYS, [tYk[k]], dma=True)
            ts("dve", accs, Yk[0], g4[:, tti, 0:1], None, ALU.mult, None, [tYk[0], tG4[tti]], [tAcc])
            for k in range(1, 4):
                stt(accs, Yk[k], g4[:, tti, k:k + 1], accs, ALU.mult, ALU.add, [tYk[k], tG4[tti], tAcc], [tAcc])
            for half in range(2):
                ps, tps = ps_next("big")
                for cc in range(4):
                    c = half * 4 + cc
                    o = ps[:, cc * 128:(cc + 1) * 128]
                    mm(o, accs[:, c * 128:(c + 1) * 128], ident_f, True, False, [tAcc, tC], [tps])
                    mm(o, aI_hi, H[:, c, tsl], False, False, [tCB, tH[tc]], [tps])
                    mm(o, aI_hi, L[:, c, tsl], False, False, [tCB, tL[tc]], [tps])
                    mm(o, aI_lo, H[:, c, tsl], False, True, [tCB, tH[tc]], [tps])
                cp("act", z2[:, half * 4:(half + 1) * 4, (tti % 4) * 128:(tti % 4 + 1) * 128],
                   ps[:].rearrange("p (c t) -> p c t", c=4), [tps], [tZ2])
            if tti % 4 == 3:
                ln_fm(z2, zb2, sd2, rstd2, tZ2, 16, 24, tc, 8, onesD_bf, True)
        if "x2" in dbg and l == 0:
            dbg_dump("x2h", H[:], [128, 8, SEQ], tH)
            dbg_dump("x2l", L[:], [128, 8, SEQ], tL)

    ob = [av(i * 4 * K, [128, D], F32) for i in range(2)]
    tOb = S.toks("ob", 2)
    tOut = S.toks("out", 2)
    S.alias(tOb, phase_toks + [t for f in range(8) for t in tMix[f]])
    for tti in range(16):
        tc = tti // 4
        tsl = slice(tti * 128, (tti + 1) * 128)
        oi = tti % 2
        for half in range(2):
            ps, tps = ps_next("big")
            for cc in range(4):
                c = half * 4 + cc
                o = ps[:, cc * 128:(cc + 1) * 128]
                mm(o, H[:, c, tsl], ident_bf, True, False, [tH[tc], tCB], [tps])
                mm(o, L[:, c, tsl], ident_bf, False, True, [tL[tc], tCB], [tps])
            cp("act", ob[oi][:, half * 512:(half + 1) * 512], ps[:], [tps], [tOb[oi]])
        S.dma("sp", out_d[tsl, :], ob[oi], [tOb[oi]], tOut[oi])
    counts = S.emit(final_toks=tOut + list(dbg_out.values()))
    return counts, S


def mybir_bf16():
    import ml_dtypes
    return ml_dtypes.bfloat16


_CACHE = {}


def kernel(x, positions, w_in, attn_gain, conv_w, conv_b, conv_ln_g, conv_ln_b, w_o, ln1_g, ln1_b, w_router, b_router,
           w_gu, b_gu, w_dn, b_dn, ln2_g, ln2_b, _nl=4, _dbg=(), _cores=8, _stop=None):
    nc = bass.Bass("TRN2", target_bir_lowering=False)
    counts, S = build(nc, _nl, _dbg, _stop, _nl)
    hc = host_consts()
    f = lambda a: np.ascontiguousarray(np.asarray(a, dtype=np.float32)[:_nl])
    shared = dict(w_in=f(w_in), attn_gain=f(attn_gain), conv_w=f(conv_w), conv_b=f(conv_b), conv_ln_g=f(conv_ln_g),
                  conv_ln_b=f(conv_ln_b), w_o=f(w_o), ln1_g=f(ln1_g), ln1_b=f(ln1_b), w_router=f(w_router), b_router=f(b_router),
                  w_gu=f(w_gu), b_gu=f(b_gu), w_dn=f(w_dn), b_dn=f(b_dn), ln2_g=f(ln2_g), ln2_b=f(ln2_b),
                  cst=hc["cst"], kaug=hc["kaug"], tokid=hc["tokid"], zero_i=hc["zero_i"])
    x = np.asarray(x, dtype=np.float32)
    positions = np.asarray(positions, dtype=np.int32)
    in_maps = []
    for b in range(_cores):
        m = dict(shared)
        m["x"] = np.ascontiguousarray(x[b])
        m["pos"] = np.ascontiguousarray(positions[b:b + 1])
        in_maps.append(m)
    res = run_bass_kernel_spmd(nc, in_maps, core_ids=list(range(_cores)))
    if _dbg:
        return res.results
    return np.stack([r["out"] for r in res.results], axis=0).astype(np.float32)
```
